# Optimizing a Trainium2 kernel written in Bass

```python
import jax, jax.numpy as jnp
from jax import lax
import numpy as np

D_MODEL = 1024
BATCH = 8
SEQ = 4096
DEPTH = 1

N_F_GROUPS = 4
F_GROUP_DIM = 128
F_WIDTH = N_F_GROUPS * F_GROUP_DIM
N_S_GROUPS = 4
S_GROUP_DIM = 128
S_WIDTH = N_S_GROUPS * S_GROUP_DIM
CHUNK = 128
IN_WIDTH = F_WIDTH + 2 * S_WIDTH + 2 * D_MODEL
N_EXPERTS = 16
CAPACITY_FACTOR = 2
D_FF_EXPERT = 2048
EPS = 1e-6

kernel_name = "hybrid_fnet_gmlp_ec_moe_block"


def rms_norm(x, g):
    xf = x.astype(jnp.float32)
    y = xf * lax.rsqrt(jnp.mean(xf * xf, axis=-1, keepdims=True) + EPS)
    return (y * g.astype(jnp.float32)).astype(x.dtype)


def fourier_mixer(z):
    b, s, _ = z.shape
    zg = z.reshape(b, s, N_F_GROUPS, F_GROUP_DIM).astype(jnp.float32)
    zf = jnp.fft.fft2(zg, axes=(1, 3), norm="ortho").real
    return zf.reshape(b, s, F_WIDTH).astype(z.dtype)


def spatial_gating_mixer(u, v, ln_g, ln_b, w_s, b_s):
    b, s, _ = u.shape
    u = jax.nn.gelu(u)
    v = jax.nn.gelu(v)
    vg = v.reshape(b, s, N_S_GROUPS, S_GROUP_DIM).astype(jnp.float32)
    mu = jnp.mean(vg, axis=-1, keepdims=True)
    var = jnp.mean(jnp.square(vg - mu), axis=-1, keepdims=True)
    vn = (vg - mu) * lax.rsqrt(var + EPS)
    vn = vn * ln_g.reshape(N_S_GROUPS, S_GROUP_DIM).astype(jnp.float32) + ln_b.reshape(N_S_GROUPS, S_GROUP_DIM).astype(jnp.float32)
    vc = vn.astype(u.dtype).reshape(b, s // CHUNK, CHUNK, N_S_GROUPS, S_GROUP_DIM)
    mixed = jnp.einsum("hpq,bnqhc->bnphc", w_s, vc) + b_s.T[None, None, :, :, None]
    return u * mixed.reshape(b, s, S_WIDTH)


def expert_choice_moe(h, w_router, w_gate_e, w_up_e, w_down_e):
    b, s, _ = h.shape
    cap = CAPACITY_FACTOR * s // N_EXPERTS
    logits = jnp.einsum("bsd,de->bse", h, w_router).astype(jnp.float32)
    affinity = jax.nn.softmax(logits, axis=-1)
    gate_val, tok_idx = lax.top_k(jnp.transpose(affinity, (0, 2, 1)), cap)
    bidx = jnp.arange(b)[:, None, None]
    xe = h[bidx, tok_idx]
    a = jnp.einsum("becd,edf->becf", xe, w_gate_e)
    g = jnp.einsum("becd,edf->becf", xe, w_up_e)
    ye = jnp.einsum("becf,efd->becd", jax.nn.silu(a) * g, w_down_e)
    ye = ye * gate_val[..., None].astype(ye.dtype)
    return jnp.zeros_like(h).at[bidx, tok_idx].add(ye)


def setup_inputs(seed: int = 0) -> dict:
    key = jax.random.key(seed)
    ks = jax.random.split(key, 20)
    nrm = lambda k, shape, scale: jax.random.normal(k, shape, jnp.float32) * scale
    L, D = DEPTH, D_MODEL
    return {
        "x": nrm(ks[0], (BATCH, SEQ, D), 1.0),
        "norm1_g": 1.0 + nrm(ks[1], (L, D), 0.1),
        "w_in": nrm(ks[2], (L, D, IN_WIDTH), D ** -0.5),
        "sgu_ln_g": 1.0 + nrm(ks[3], (L, S_WIDTH), 0.1),
        "sgu_ln_b": nrm(ks[4], (L, S_WIDTH), 0.1),
        "w_spatial": nrm(ks[5], (L, N_S_GROUPS, CHUNK, CHUNK), CHUNK ** -0.5),
        "b_spatial": 1.0 + nrm(ks[6], (L, N_S_GROUPS, CHUNK), 0.1),
        "w_fourier_out": nrm(ks[7], (L, F_WIDTH, D), F_WIDTH ** -0.5),
        "w_sgu_out": nrm(ks[8], (L, S_WIDTH, D), S_WIDTH ** -0.5),
        "w_out": nrm(ks[9], (L, D, D), D ** -0.5),
        "norm2_g": 1.0 + nrm(ks[10], (L, D), 0.1),
        "w_router": nrm(ks[11], (L, D, N_EXPERTS), D ** -0.5),
        "w_gate_e": nrm(ks[12], (L, N_EXPERTS, D, D_FF_EXPERT), D ** -0.5),
        "w_up_e": nrm(ks[13], (L, N_EXPERTS, D, D_FF_EXPERT), D ** -0.5),
        "w_down_e": nrm(ks[14], (L, N_EXPERTS, D_FF_EXPERT, D), D_FF_EXPERT ** -0.5),
        "final_g": 1.0 + nrm(ks[15], (D,), 0.1),
    }


def reference(x, norm1_g, w_in, sgu_ln_g, sgu_ln_b, w_spatial, b_spatial, w_fourier_out, w_sgu_out, w_out,
              norm2_g, w_router, w_gate_e, w_up_e, w_down_e, final_g):
    cuts = [F_WIDTH, F_WIDTH + S_WIDTH, F_WIDTH + 2 * S_WIDTH, F_WIDTH + 2 * S_WIDTH + D_MODEL]
    for l in range(DEPTH):
        h = rms_norm(x, norm1_g[l])
        p = jnp.einsum("bsd,dk->bsk", h, w_in[l])
        z_f, u, v, g_f, g_s = jnp.split(p, cuts, axis=-1)
        y_f = jnp.einsum("bsk,kd->bsd", fourier_mixer(z_f), w_fourier_out[l])
        y_s = jnp.einsum("bsk,kd->bsd",
                         spatial_gating_mixer(u, v, sgu_ln_g[l], sgu_ln_b[l], w_spatial[l], b_spatial[l]),
                         w_sgu_out[l])
        merged = jax.nn.sigmoid(g_f) * y_f + jax.nn.sigmoid(g_s) * y_s
        x = x + jnp.einsum("bsd,de->bse", merged, w_out[l])
        h2 = rms_norm(x, norm2_g[l])
        x = x + expert_choice_moe(h2, w_router[l], w_gate_e[l], w_up_e[l], w_down_e[l])
    return rms_norm(x, final_g)
```

```python
from contextlib import ExitStack
import numpy as np
import ml_dtypes
import concourse.bass as bass
import concourse.mybir as mybir
from concourse.bass_utils import run_bass_kernel_spmd

F32 = mybir.dt.float32
BF16 = mybir.dt.bfloat16
I32 = mybir.dt.int32
ALU = mybir.AluOpType
AF = mybir.ActivationFunctionType
AX = mybir.AxisListType

ENGS = ("pe", "act", "dve", "pool", "sp")
EPS = 1e-6
NTOK = 4096
D = 1024
NE = 16
CAP = 512


class Res:
    __slots__ = ("w", "r", "name")

    def __init__(self, name=""):
        self.w = None
        self.r = []
        self.name = name


class Prog:
    def __init__(self, nc, stack):
        self.nc = nc
        self.stack = stack
        self.ops = {e: [] for e in ENGS}
        self.cnt = {e: 0 for e in ENGS}
        self.pending = {e: False for e in ENGS}
        self.sems = {}
        self.dcnt = {}
        self.seen = {e: {} for e in ENGS}
        for e in ENGS:
            self.sems[e] = stack.enter_context(nc.semaphore("s_" + e))

    def dsem(self, name):
        if name not in self.sems:
            self.sems[name] = self.stack.enter_context(self.nc.semaphore("d_" + name))
            self.dcnt[name] = 0
        return name

    def _wait(self, eng, k, v):
        if self.seen[eng].get(k, 0) < v:
            self.seen[eng][k] = v
            self.ops[eng].append(("wait", k, v))

    def _deps(self, eng, reads, writes):
        deps = {}

        def add(tok):
            if tok is None:
                return
            k, v = tok
            if k == eng and eng == "pe":
                return
            if deps.get(k, 0) < v:
                deps[k] = v

        for b in reads:
            add(b.w)
        for b in writes:
            add(b.w)
            for t in b.r:
                add(t)
        for k, v in deps.items():
            self._wait(eng, k, v)

    def _commit(self, tok, reads, writes):
        for b in reads:
            b.r.append(tok)
        for b in writes:
            b.w = tok
            b.r = []

    def op(self, eng, fn, reads=(), writes=(), inc=True):
        self._deps(eng, reads, writes)
        if inc:
            self.cnt[eng] += 1
            self.pending[eng] = False
            tok = (eng, self.cnt[eng])
            self.ops[eng].append(("op", fn, eng, 1))
        else:
            self.pending[eng] = True
            tok = (eng, self.cnt[eng] + 1)
            self.ops[eng].append(("op", fn, None, 0))
        self._commit(tok, reads, writes)
        return tok

    def dma(self, q, fn, sem, reads=(), writes=()):
        self.dsem(sem)
        self._deps(q, reads, writes)
        self.dcnt[sem] += 16
        tok = (sem, self.dcnt[sem])
        self.ops[q].append(("op", fn, sem, 16))
        self._commit(tok, reads, writes)
        return tok

    def barrier(self):
        assert not any(self.pending.values())
        for e in ENGS:
            for k in ENGS:
                if k != e and self.cnt[k] > 0:
                    self._wait(e, k, self.cnt[k])
            for k, v in self.dcnt.items():
                if v > 0:
                    self._wait(e, k, v)

    def flush_one(self, e, h):
        for o in self.ops[e]:
            if o[0] == "wait":
                h.wait_ge(self.sems[o[1]], o[2])
            else:
                ins = o[1](h)
                if o[2] is not None:
                    ins.then_inc(self.sems[o[2]], o[3])


class _Stop(Exception):
    pass


def build(dbg=False, stop=None):
    nc = bass.Bass("TRN2", target_bir_lowering=False)

    def din(name, shape, dt=F32):
        return nc.dram_tensor(name, shape, dt, kind="ExternalInput").ap()

    x = din("x", [NTOK, D])
    w_in = din("w_in", [D, 3584])
    wfo = din("w_fo", [512, D])
    wso = din("w_so", [512, D])
    wout = din("w_out", [D, D])
    wr = din("w_r", [D, NE])
    wg = din("wg", [NE * 8 * 128, 2048])
    wu = din("wu", [NE * 8 * 128, 2048])
    wd = din("wd", [NE, 2048, D])
    g1 = din("g1", [D])
    g2 = din("g2", [D])
    fg = din("fg", [D])
    lng = din("lng", [512])
    lnb = din("lnb", [512])
    wsT_d = din("wsT", [128, 512])
    bs_d = din("bs", [1, 512])
    cbf_d = din("cbf", [128, 768], BF16)
    cf_d = din("cf", [128, 448], F32)
    dC = din("dC", [2048, 2048], BF16)
    dCr = din("dCr", [1, 2048], BF16)
    dC8 = din("dC8", [128, 34], BF16)
    dS = din("dS", [2048, 2048], BF16)
    out = nc.dram_tensor("out", [NTOK, D], F32, kind="ExternalOutput").ap()
    acc = nc.dram_tensor("acc", [NTOK, D], F32).ap()
    xh2d = nc.dram_tensor("xh2d", [NTOK, D], BF16).ap()
    zfd = nc.dram_tensor("zfd", [128, 4 * NTOK], BF16).ap()
    dbg_out = {}
    if dbg:
        for nm, shp in (("d_zf", [128, 4 * 4096]), ("d_aff", [128, 512]), ("d_idx", [128, 64]), ("d_gate", [128, 64]),
                        ("d_thr", [16, 4])):
            dbg_out[nm] = nc.dram_tensor(nm, shp, F32, kind="ExternalOutput").ap()

    with ExitStack() as st:
        P = Prog(nc, st)
        def A(name, shape, dt):
            return nc.alloc_sbuf_tensor("sb_" + name, shape, dt)

        cbf = A("cbf", [128, 768], BF16)
        ident, tri, onesb, Cc, Sc, iotaB = (cbf[:, i * 128:(i + 1) * 128] for i in range(6))
        cf = A("cf", [128, 448], F32)
        identF, iotaF, thi, tlo, Bsum = cf[:, 0:128], cf[:, 128:256], cf[:, 256:288], cf[:, 288:320], cf[:, 320:448]
        g1bc = A("g1bc", [128, D], F32)
        g2bc = A("g2bc", [128, D], F32)
        lngbc = A("lngbc", [128, 512], F32)
        lnbbc = A("lnbbc", [128, 512], F32)
        wsTb = A("wsTb", [128, 512], BF16)
        bsb = A("bsb", [1, 512], BF16)
        wrb = A("wrb", [128, 8, NE], BF16)
        aff = A("aff", [128, 32, NE], F32)
        gates = A("gates", [128, NE, 4], F32)
        idxs = A("idxs", [128, NE, 4], I32)
        small = A("small", [128, 64], F32)
        onesF = A("onesF", [128, 128], F32)
        mhalf = A("mhalf", [128, 4], F32)
        ARENA_W = 47104
        arena = A("arena", [128, ARENA_W], F32)
        R_const = Res("const")

        def carve(off_b, shape, dt):
            n = int(np.prod(shape[1:]))
            esz = 2 if dt == BF16 else 4
            assert off_b % 4 == 0 and off_b + n * esz <= ARENA_W * 4, (off_b, shape)
            w0 = off_b // 4
            w1 = w0 + (n * esz + 3) // 4
            ap = arena[0:shape[0], w0:w1]
            if dt != F32:
                ap = ap.bitcast(dt)
            ap = ap[:, 0:n]
            if len(shape) == 3:
                ap = ap.rearrange("p (a b) -> p a b", a=shape[1])
            elif len(shape) == 4:
                ap = ap.rearrange("p (a b c) -> p a b c", a=shape[1], b=shape[2])
            return ap

        banks = [nc.alloc_psum_tensor("bank%d" % i, [128, 512], F32) for i in range(8)]
        bankR = [Res("bank%d" % i) for i in range(8)]
        bstate = {"i": 0}

        def nb():
            i = bstate["i"]
            bstate["i"] = (i + 1) % 8
            return banks[i], bankR[i]

        def mm(o, lhsT, rhs, start, stop, reads, writes, inc):
            P.op("pe", lambda e: e.matmul(o, lhsT, rhs, start=start, stop=stop), reads, writes, inc=inc)

        def act(o, i, func, reads, writes, **kw):
            P.op("act", lambda e: e.activation(out=o, in_=i, func=func, **kw), reads, writes)

        def tt(eng, o, a, b, op, reads, writes):
            P.op(eng, lambda e: e.tensor_tensor(out=o, in0=a, in1=b, op=op), reads, writes)

        def ts(eng, o, a, s1, s2, op0, op1, reads, writes, accum_out=None):
            if accum_out is None:
                P.op(eng, lambda e: e.tensor_scalar(out=o, in0=a, scalar1=s1, scalar2=s2, op0=op0, op1=op1), reads, writes)
            else:
                P.op(eng, lambda e: e.tensor_scalar(out=o, in0=a, scalar1=s1, scalar2=s2, op0=op0, op1=op1,
                                                    accum_out=accum_out), reads, writes)

        def ts1(eng, o, a, s1, op0, reads, writes):
            P.op(eng, lambda e: e.tensor_scalar(out=o, in0=a, scalar1=s1, scalar2=None, op0=op0), reads, writes)

        def stt(eng, o, a, s, b, op0, op1, reads, writes):
            P.op(eng, lambda e: e.scalar_tensor_tensor(out=o, in0=a, scalar=s, in1=b, op0=op0, op1=op1), reads, writes)

        def cp(eng, o, i, reads, writes):
            if eng == "act":
                P.op("act", lambda e: e.activation(out=o, in_=i, func=AF.Copy), reads, writes)
            else:
                P.op(eng, lambda e: e.tensor_copy(out=o, in_=i), reads, writes)

        def red(eng, o, i, op, reads, writes):
            P.op(eng, lambda e: e.tensor_reduce(out=o, in_=i, axis=AX.X, op=op), reads, writes)

        def recip(o, i, reads, writes):
            P.op("dve", lambda e: e.reciprocal(out=o, in_=i), reads, writes)

        def dma(q, o, i, sem, reads, writes):
            return P.dma(q, lambda e: e.dma_start(out=o, in_=i), sem, reads, writes)

        def finish():
            with nc.Block() as block:
                @block.sync
                def _(e):
                    P.flush_one("sp", e)

                @block.scalar
                def _(e):
                    P.flush_one("act", e)

                @block.vector
                def _(e):
                    P.flush_one("dve", e)

                @block.gpsimd
                def _(e):
                    P.flush_one("pool", e)

                @block.tensor
                def _(e):
                    P.flush_one("pe", e)

        dma("sp", cbf[:], cbf_d, "c0", [], [R_const])
        dma("sp", cf[:], cf_d, "c0", [], [R_const])
        dma("sp", g1bc[:], g1.partition_broadcast(128), "c0", [], [R_const])
        dma("sp", g2bc[:], g2.partition_broadcast(128), "c0", [], [R_const])
        dma("sp", lngbc[:], lng.partition_broadcast(128), "c0", [], [R_const])
        dma("sp", lnbbc[:], lnb.partition_broadcast(128), "c0", [], [R_const])
        R_c2 = Res("const2")
        dma("pool", wsTb[:], wsT_d, "c1", [], [R_c2])
        dma("pool", bsb[:], bs_d, "c1", [], [R_c2])
        dma("pool", wrb[:], wr.rearrange("(c p) e -> p c e", p=128), "c1", [], [R_c2])
        P.op("dve", lambda e: e.memset(onesF[:], 1.0), [], [R_c2])
        P.op("dve", lambda e: e.memset(mhalf[:], -0.5), [], [R_c2])
        CR = [R_const, R_c2]

        def rms_head(xt_ap, xt_res, junk_ap, col, **jkw):
            ss = small[:, col:col + 1]
            rs = small[:, col + 1:col + 2]
            R_s = Res("s")
            act(junk_ap, xt_ap, AF.Square, [xt_res], [R_s], scale=1.0 / 32.0, accum_out=ss, **jkw)
            act(ss, ss, AF.Identity, [R_s], [R_s], scale=1.0, bias=EPS)
            P.op("pool", lambda e: e.tensor_tensor(out=rs, in0=ss, in1=mhalf[:, 0:1], op=ALU.pow), [R_s] + CR, [R_s])
            return R_s

        def rms_tail(xt_ap, xt_res, xh_ap, xh_res, gbc, col, R_s):
            rs = small[:, col + 1:col + 2]
            stt("dve", xh_ap, xt_ap, rs, gbc, ALU.mult, ALU.mult, [xt_res, R_s] + CR, [xh_res])

        def transposes(src_ap, src_res, dst_ap, dst_res, i, evac_engs=("act", "dve")):
            for h in range(2):
                bk, br = nb()
                for q in range(4):
                    c = h * 4 + q
                    mm(bk[:, q * 128:(q + 1) * 128], src_ap[:, c * 128:(c + 1) * 128], ident, True, True,
                       [src_res] + CR, [br], q == 3)
                cp(evac_engs[h], dst_ap[:, h * 4:(h + 1) * 4, i * 128:(i + 1) * 128],
                   bk[:, :].rearrange("p (a b) -> p a b", a=4), [br], [dst_res])

        o = 0
        zT = carve(o, [128, 4, 4096], F32); o += 65536
        ze = carve(o, [128, 4, 2052], BF16); o += 4 * 2052 * 2
        zo = carve(o, [128, 4, 2052], BF16); o += 4 * 2052 * 2
        O_SMALL = o
        xt_s = [carve(o + i * 4096, [128, D], F32) for i in range(4)]; o += 4 * 4096
        xh_s = [carve(o + i * 2048, [128, D], BF16) for i in range(4)]; o += 4 * 2048
        xhT_s = [carve(o + i * 8192, [128, 8, 512], BF16) for i in range(2)]; o += 2 * 8192
        WfB = carve(o, [128, 8, 512], BF16); o += 8192
        junkA = carve(o, [128, D], BF16); o += 2048
        assert o <= ARENA_W * 4
        xt_r = [Res() for _ in range(4)]
        xh_r = [Res() for _ in range(4)]
        xhT_r = [Res() for _ in range(2)]
        R_wf = Res()
        R_zT = Res()
        w_in_v = w_in.rearrange("(c p) k -> p c k", p=128)
        dma("pool", WfB[:, :, :], w_in_v[:, :, 0:512], "wf", [], [R_wf])

        def load_x(j):
            slot = j % 4
            dma("sp", xt_s[slot][:, :], x[j * 128:(j + 1) * 128, :], "xt%d" % slot, [], [xt_r[slot]])

        rsA = {}

        def headA(j):
            sl = j % 4
            rsA[j] = rms_head(xt_s[sl][:, :], xt_r[sl], junkA[:, :], (j % 8) * 2)

        def tailA(j):
            sl, hs = j % 4, j % 4
            rms_tail(xt_s[sl][:, :], xt_r[sl], xh_s[hs][:, :], xh_r[hs], g1bc[:, :], (j % 8) * 2, rsA.pop(j))

        pendZ = []

        def transA(j):
            sti, i = divmod(j, 4)
            ts_ = sti % 2
            transposes(xh_s[j % 4], xh_r[j % 4], xhT_s[ts_], xhT_r[ts_], i)
            if i == 3:
                for g in range(4):
                    pendZ.append((j + 4 + g, sti, g))

        def zgroup(it, sti, g):
            ts_ = sti % 2
            bk, br = nb()
            for c in range(8):
                mm(bk[:, :], WfB[:, c, g * 128:(g + 1) * 128], xhT_s[ts_][:, c, :], c == 0, c == 7,
                   [R_wf, xhT_r[ts_]], [br], c == 7)
            deferredA.append((it + 1, "act" if g % 2 == 0 else "dve", zT[:, g, sti * 512:(sti + 1) * 512], bk, br))

        for j in range(3):
            load_x(j)
        deferredA = []
        for j in range(44):
            if j < 32:
                headA(j)
            if 0 <= j - 1 < 32:
                tailA(j - 1)
            if j + 3 < 32:
                load_x(j + 3)
            while deferredA and deferredA[0][0] <= j:
                _, eng_, dst_, bk_, br_ = deferredA.pop(0)
                cp(eng_, dst_, bk_[:, :], [br_], [R_zT])
            if 0 <= j - 4 < 32:
                transA(j - 4)
            while pendZ and pendZ[0][0] <= j:
                _, sti_, g_ = pendZ.pop(0)
                zgroup(j, sti_, g_)
        assert not deferredA and not pendZ
        R_ze = Res()
        R_zo = Res()
        tt("dve", ze[:, :, 1:2048], zT[:, :, 1:2048], zT[:, :, 4095:2048:-1], ALU.add, [R_zT], [R_ze])
        cp("dve", ze[:, :, 0:1], zT[:, :, 0:1], [R_zT], [R_ze])
        cp("dve", ze[:, :, 2048:2049], zT[:, :, 2048:2049], [R_zT], [R_ze])
        tt("dve", zo[:, :, 1:2048], zT[:, :, 1:2048], zT[:, :, 4095:2048:-1], ALU.subtract, [R_zT], [R_zo])
        P.op("dve", lambda e: e.memset(zo[:, :, 0:1], 0.0), [], [R_zo])
        P.barrier()

        if stop == "A":
            finish()
            return nc
        o = O_SMALL
        Y = carve(o, [128, 17, 2, 512], BF16); o += 17 * 2 * 512 * 2
        dftC = [carve(o + i * 8704, [128, 17, 256], BF16) for i in range(2)]; o += 2 * 8704
        dftS = [carve(o + i * 8192, [128, 16, 256], BF16) for i in range(2)]; o += 2 * 8192
        tmpB = [carve(o + i * 1024, [128, 256], F32) for i in range(2)]; o += 2048
        c8 = carve(o, [128, 17, 2], BF16); o += 68
        O_WOUT = 169984
        assert o <= O_WOUT
        zfT = carve(0, [128, 4, 4096], BF16)
        O_W2 = 32768
        Win2 = carve(O_W2, [128, 8, 3072], BF16)
        WfoB = carve(O_W2 + 49152, [128, 4, D], BF16)
        WsoB = carve(O_W2 + 57344, [128, 4, D], BF16)
        assert O_W2 + 65536 <= O_SMALL
        WoutB = carve(O_WOUT, [128, 8, D], BF16)
        R_Y = [Res() for _ in range(17)]
        R_zf = Res()
        dft_r = [Res() for _ in range(2)]
        tmpB_r = [Res() for _ in range(2)]
        R_c8 = Res()
        R_w2 = Res()
        dC_v = dC.rearrange("(t p) k -> p t k", p=128)
        dS_v = dS.rearrange("(t p) k -> p t k", p=128)

        def load_dft(kb):
            s = kb % 2
            k0 = kb * 256
            dma("sp", dftC[s][:, 0:16, :], dC_v[:, :, k0:k0 + 256], "dft%d" % s, [], [dft_r[s]])
            dma("sp", dftC[s][0:1, 16, :], dCr[0:1, k0:k0 + 256], "dft%d" % s, [], [dft_r[s]])
            dma("sp", dftS[s][:, :, :], dS_v[:, :, k0:k0 + 256], "dft%d" % s, [], [dft_r[s]])

        load_dft(0)
        load_dft(1)
        dma("sp", c8[:, :, :], dC8.rearrange("p (t w) -> p t w", w=2), "c8", [], [R_c8])
        for nt in range(17):
            rows = 128 if nt < 16 else 1
            bk, br = nb()
            for g in range(4):
                mm(bk[0:rows, g * 128:(g + 1) * 128], ze[:, g, nt * 128:nt * 128 + rows], Cc, True, True,
                   [R_ze] + CR, [br], g == 3)
            cp("act", Y[0:rows, nt, 0, :], bk[0:rows, :], [br], [R_Y[nt]])
            if nt < 16:
                bk, br = nb()
                for g in range(4):
                    mm(bk[:, g * 128:(g + 1) * 128], zo[:, g, nt * 128:(nt + 1) * 128], Sc, True, True,
                       [R_zo] + CR, [br], g == 3)
                cp("dve", Y[:, nt, 1, :], bk[:, :], [br], [R_Y[nt]])
        def prefetch_w2():
            for c in range(8):
                for h in range(2):
                    dma("pool", Win2[:, c, h * 1536:(h + 1) * 1536], w_in_v[:, c, 512 + h * 1536:512 + (h + 1) * 1536], "w2",
                        [], [R_w2, R_ze, R_zo])
            dma("pool", WfoB[:, :, :], wfo.rearrange("(c p) k -> p c k", p=128), "w2", [], [R_w2, R_ze, R_zo])
            dma("pool", WsoB[:, :, :], wso.rearrange("(c p) k -> p c k", p=128), "w2", [], [R_w2, R_ze, R_zo])
            dma("pool", WoutB[:, :, :], wout.rearrange("(c p) k -> p c k", p=128), "w2", [], [R_w2])

        for kb in range(8):
            s = kb % 2
            k0 = kb * 256
            for g in range(4):
                bA, rA = nb()
                for nt in range(17):
                    rows = 128 if nt < 16 else 1
                    mm(bA[:, 0:256], Y[0:rows, nt, 0, g * 128:(g + 1) * 128], dftC[s][0:rows, nt, :], nt == 0, nt == 16,
                       [R_Y[nt], dft_r[s]], [rA], nt == 16)
                bB, rB = nb()
                for nt in range(16):
                    mm(bB[:, 0:256], Y[:, nt, 1, g * 128:(g + 1) * 128], dftS[s][:, nt, :], nt == 0, nt == 15,
                       [R_Y[nt], dft_r[s]], [rB], nt == 15)
                tb = (kb * 4 + g) % 2
                cp("act", tmpB[tb][:, :], bB[:, 0:256], [rB], [tmpB_r[tb]])
                tt("dve", zfT[:, g, k0:k0 + 256], bA[:, 0:256], tmpB[tb][:, :], ALU.subtract, [rA, tmpB_r[tb]], [R_zf])
                lo = 1 if kb == 0 else 0
                tt("dve", zfT[:, g, NTOK - k0 - lo:NTOK - k0 - 256:-1], bA[:, lo:256], tmpB[tb][:, lo:256], ALU.add,
                   [rA, tmpB_r[tb]], [R_zf])
            if kb + 2 < 8:
                load_dft(kb + 2)
            if kb == 1:
                prefetch_w2()
        for g in range(4):
            bA, rA = nb()
            for nt in range(17):
                rows = 128 if nt < 16 else 1
                mm(bA[:, 0:2], Y[0:rows, nt, 0, g * 128:(g + 1) * 128], c8[0:rows, nt, :], nt == 0, nt == 16,
                   [R_Y[nt], R_c8], [rA], nt == 16)
            cp("act", zfT[:, g, 2048:2049], bA[:, 0:1], [rA], [R_zf])
        zf_flat = zfT[:, :, :].rearrange("p a b -> p (a b)")
        dma("sp", zfd, zf_flat, "zfw", [R_zf], [])
        if dbg:
            zdbg = carve(O_SMALL, [128, 4 * 4096], F32)
            R_d = Res()
            P.barrier()
            cp("dve", zdbg[:, :], zf_flat, [R_zf], [R_d])
            dma("sp", dbg_out["d_zf"], zdbg[:, :], "dbg", [R_d], [])
        P.barrier()

        if stop == "B":
            finish()
            return nc
        zfd_v = zfd.rearrange("p (a b) -> p a b", a=4)
        o = 0
        zfs = [carve(o + i * 4096, [128, 4, 512], BF16) for i in range(2)]; o += 8192
        xn = [carve(o + i * 4096, [128, D], F32) for i in range(3)]; o += 12288
        xr = [carve(o + i * 4096, [128, D], F32) for i in range(2)]; o += 8192
        xhc = [carve(o + i * 2048, [128, D], BF16) for i in range(2)]; o += 4096
        assert o <= 32768
        o = O_SMALL
        xhT2 = [carve(o + i * 8192, [128, 8, 512], BF16) for i in range(2)]; o += 16384
        vn42 = [carve(o + i * 4096, [128, 4, 512], BF16) for i in range(2)]; o += 8192
        ug2 = [carve(o + i * 4096, [128, 4, 512], BF16) for i in range(2)]; o += 8192
        sT = carve(o, [128, 4, 512], BF16); o += 4096
        sfs = [carve(o + i * 1024, [128, 512], BF16) for i in range(4)]; o += 4096
        t12 = [carve(o + i * 2048, [128, 512], F32) for i in range(2)]; o += 4096
        vgb = carve(o, [128, 4, 128], F32); o += 2048
        sqj = carve(o, [128, 512], BF16)
        junk8 = arena[:, o // 4:o // 4 + 256].bitcast(mybir.dt.float8e4)
        o += 1024
        mergedT = carve(o, [128, 8, 512], BF16); o += 8192
        x1t_s = [carve(o + i * 4096, [128, D], F32) for i in range(2)]; o += 8192
        xh2_s = [carve(o + i * 2048, [128, D], BF16) for i in range(2)]; o += 4096
        xh2T = carve(o, [128, 8, 128], BF16); o += 2048
        ex = carve(o, [128, NE], F32); o += 64
        ex2 = carve(o, [128, NE], F32); o += 64
        assert o <= O_WOUT, o
        zfs_r = [Res() for _ in range(2)]
        xn_r = [Res() for _ in range(3)]
        xr_r = [Res() for _ in range(2)]
        xhc_r = [Res() for _ in range(2)]
        xhT2_r = [Res() for _ in range(2)]
        vn_r = [[Res() for _ in range(4)] for _ in range(2)]
        ug_r = [[Res() for _ in range(4)] for _ in range(2)]
        sT_r = [Res() for _ in range(4)]
        sfs_r = [Res() for _ in range(4)]
        t12_r = [Res() for _ in range(2)]
        R_vg = Res()
        mg_r = [Res() for _ in range(8)]
        R_x1s = [Res(), Res()]
        R_xh2s = [Res(), Res()]
        R_xh2T = Res()
        R_ex = Res()
        R_aff = Res()
        acc_r = [Res() for _ in range(32)]
        xh2d_r = [Res() for _ in range(32)]
        UO, VO, GFO, GSO = 0, 512, 1024, 2048

        def load_xn(j):
            sl = j % 3
            dma("sp", xn[sl][:, :], x[j * 128:(j + 1) * 128, :], "xt%d" % sl, [], [xn_r[sl]])

        def load_xr(j):
            sl = j % 2
            dma("sp", xr[sl][:, :], x[j * 128:(j + 1) * 128, :], "xr%d" % sl, [], [xr_r[sl]])

        def part1(st):
            s2 = st % 2
            n0 = st * 512

            rsC = {}

            def hA(i):
                def f():
                    j = st * 4 + i
                    if i == 0:
                        dma("sp", zfs[s2][:, :, :], zfd_v[:, :, n0:n0 + 512], "zfs%d" % s2, [], [zfs_r[s2]])
                    rsC[i] = rms_head(xn[j % 3][:, :], xn_r[j % 3], junk8, 16 + (j % 8) * 2, saturate=False)
                return f

            def tA(i):
                def f():
                    j = st * 4 + i
                    rms_tail(xn[j % 3][:, :], xn_r[j % 3], xhc[i % 2][:, :], xhc_r[i % 2], g1bc[:, :], 16 + (j % 8) * 2, rsC.pop(i))
                    if j + 3 < 32:
                        load_xn(j + 3)
                return f

            def cB(i):
                def f():
                    transposes(xhc[i % 2], xhc_r[i % 2], xhT2[s2], xhT2_r[s2], i)
                return f

            def cVa(i):
                def f():
                    bk, br = nb()
                    for c in range(8):
                        mm(bk[:, :], xhT2[s2][:, c, i * 128:(i + 1) * 128], Win2[:, c, VO:VO + 512], c == 0, c == 7,
                           [xhT2_r[s2], R_w2], [br], c == 7)
                    vg2 = vgb[:, :, :].rearrange("p a b -> p (a b)")
                    act(vg2, bk[:, :], AF.Gelu_apprx_tanh, [br], [R_vg])
                    s1 = small[:, 40:44]
                    s2c = small[:, 44:48]
                    red("dve", s1, vgb[:, :, :], ALU.add, [R_vg], [R_vg])
                    ts1("dve", s1, s1, 1.0 / 128, ALU.mult, [R_vg], [R_vg])
                    tt("dve", vgb[:, :, :], vgb[:, :, :], s1.unsqueeze(2).to_broadcast([128, 4, 128]), ALU.subtract, [R_vg], [R_vg])
                    for g in range(4):
                        act(sqj[:, g * 128:(g + 1) * 128], vgb[:, g, :], AF.Square, [R_vg], [R_vg], scale=float(128.0 ** -0.5),
                            accum_out=s2c[:, g:g + 1])
                    act(s2c, s2c, AF.Identity, [R_vg], [R_vg], scale=1.0, bias=EPS)
                    P.op("pool", lambda e: e.tensor_tensor(out=s2c, in0=s2c, in1=mhalf[:, 0:4], op=ALU.pow), [R_vg] + CR, [R_vg])
                return f

            def cVb(i):
                def f():
                    vg2 = vgb[:, :, :].rearrange("p a b -> p (a b)")
                    s2c = small[:, 44:48]
                    for g in range(4):
                        stt("dve", vgb[:, g, :], vgb[:, g, :], s2c[:, g:g + 1], lngbc[:, g * 128:(g + 1) * 128], ALU.mult, ALU.mult,
                            [R_vg] + CR, [R_vg])
                    tt("dve", vn42[s2][:, i, :], vg2, lnbbc[:, :], ALU.add, [R_vg] + CR, [vn_r[s2][i]])
                return f

            def cU(cb):
                def f():
                    bk, br = nb()
                    for c in range(8):
                        mm(bk[:, :], Win2[:, c, UO + cb * 128:UO + (cb + 1) * 128], xhT2[s2][:, c, :], c == 0, c == 7,
                           [xhT2_r[s2], R_w2], [br], c == 7)
                    act(ug2[s2][:, cb, :], bk[:, :], AF.Gelu_apprx_tanh, [br], [ug_r[s2][cb]])
                return f

            def seq(*fs):
                def f():
                    for g_ in fs:
                        g_()
                return f

            nop = lambda: None
            AB = [hA(0), seq(hA(1), tA(0)), seq(hA(2), tA(1), cB(0)), seq(hA(3), tA(2), cB(1)), seq(tA(3), cB(2)), cB(3), nop, nop]
            return dict(AB=AB, Va=[cVa(i) for i in range(4)], Vb=[cVb(i) for i in range(4)], U=[cU(cb) for cb in range(4)])

        def part2(st):
            s2 = st % 2
            n0 = st * 512

            def cS(g):
                def f():
                    bk, br = nb()
                    for i in range(4):
                        mm(bk[:, i * 128:(i + 1) * 128], vn42[s2][:, i, g * 128:(g + 1) * 128], wsTb[:, g * 128:(g + 1) * 128],
                           True, False, [vn_r[s2][i]] + CR, [br], False)
                        mm(bk[:, i * 128:(i + 1) * 128], onesb[0:1, :], bsb[0:1, g * 128:(g + 1) * 128],
                           False, True, CR, [br], i == 3)
                    tt("dve", sT[:, g, :], bk[:, :], ug2[s2][:, g, :], ALU.mult, [br, ug_r[s2][g]], [sT_r[g]])
                return f

            def cG(db):
                def f():
                    bgf, rgf = nb()
                    for c in range(8):
                        mm(bgf[:, :], Win2[:, c, GFO + db * 128:GFO + (db + 1) * 128], xhT2[s2][:, c, :], c == 0, c == 7,
                           [xhT2_r[s2], R_w2], [rgf], c == 7)
                    bgs, rgs = nb()
                    for c in range(8):
                        mm(bgs[:, :], Win2[:, c, GSO + db * 128:GSO + (db + 1) * 128], xhT2[s2][:, c, :], c == 0, c == 7,
                           [xhT2_r[s2], R_w2], [rgs], c == 7)
                    byf, ryf = nb()
                    for g in range(4):
                        mm(byf[:, :], WfoB[:, g, db * 128:(db + 1) * 128], zfs[s2][:, g, :], g == 0, g == 3,
                           [zfs_r[s2], R_w2], [ryf], g == 3)
                    bys, rys = nb()
                    for g in range(4):
                        mm(bys[:, :], WsoB[:, g, db * 128:(db + 1) * 128], sT[:, g, :], g == 0, g == 3,
                           [sT_r[g], R_w2], [rys], g == 3)
                    q = (db % 2) * 2
                    act(sfs[q][:, :], bgf[:, :], AF.Sigmoid, [rgf], [sfs_r[q]])
                    act(sfs[q + 1][:, :], bgs[:, :], AF.Sigmoid, [rgs], [sfs_r[q + 1]])
                    tt("dve", t12[0][:, :], byf[:, :], sfs[q][:, :], ALU.mult, [ryf, sfs_r[q]], [t12_r[0]])
                    tt("dve", t12[1][:, :], bys[:, :], sfs[q + 1][:, :], ALU.mult, [rys, sfs_r[q + 1]], [t12_r[1]])
                    tt("pool", mergedT[:, db, :], t12[0][:, :], t12[1][:, :], ALU.add, [t12_r[0], t12_r[1]], [mg_r[db]])
                return f

            rsX = {}

            def cX1(i):
                def f():
                    j = st * 4 + i
                    sl = j % 2
                    x1t, xh2, R_x1, R_xh2 = x1t_s[sl], xh2_s[sl], R_x1s[sl], R_xh2s[sl]
                    for hf in range(2):
                        bk, br = nb()
                        for c in range(8):
                            mm(bk[:, :], mergedT[:, c, i * 128:(i + 1) * 128], WoutB[:, c, hf * 512:(hf + 1) * 512], c == 0, c == 7,
                               [mg_r[c], R_w2], [br], c == 7)
                        tt("dve", x1t[:, hf * 512:(hf + 1) * 512], bk[:, :], xr[sl][:, hf * 512:(hf + 1) * 512], ALU.add,
                           [br, xr_r[sl]], [R_x1])
                    if j + 2 < 32:
                        load_xr(j + 2)
                    dma("sp", acc[j * 128:(j + 1) * 128, :], x1t[:, :], "accw", [R_x1], [acc_r[j]])
                    rsX[i] = rms_head(x1t[:, :], R_x1, junk8, 32 + (j % 4) * 2, saturate=False)
                return f

            def cXT(i):
                def f():
                    j = st * 4 + i
                    sl = j % 2
                    x1t, xh2, R_x1, R_xh2 = x1t_s[sl], xh2_s[sl], R_x1s[sl], R_xh2s[sl]
                    rms_tail(x1t[:, :], R_x1, xh2[:, :], R_xh2, g2bc[:, :], 32 + (j % 4) * 2, rsX.pop(i))
                    dma("sp", xh2d[j * 128:(j + 1) * 128, :], xh2[:, :], "xh2w", [R_xh2], [xh2d_r[j]])
                return f

            def cX2a(i):
                def f():
                    j = st * 4 + i
                    sl = j % 2
                    x1t, xh2, R_x1, R_xh2 = x1t_s[sl], xh2_s[sl], R_x1s[sl], R_xh2s[sl]
                    transposes(xh2, R_xh2, xh2T, R_xh2T, 0)
                return f

            def cX2l(i):
                def f():
                    bk, br = nb()
                    for c in range(8):
                        mm(bk[:, 0:NE], xh2T[:, c, :], wrb[:, c, :], c == 0, c == 7, [R_xh2T] + CR, [br], c == 7)
                    mx = small[:, 48:49]
                    red("dve", mx, bk[:, 0:NE], ALU.max, [br], [R_ex])
                    ts1("dve", mx, mx, -0.5, ALU.mult, [R_ex], [R_ex])
                    act(ex[:, :], bk[:, 0:NE], AF.Tanh, [br, R_ex], [R_ex], bias=mx, scale=0.5)
                return f

            def cX2b(i):
                def f():
                    j = st * 4 + i
                    sm = small[:, 49:50]
                    ts("dve", ex2[:, :], ex[:, :], -1.0, 1.0, ALU.mult, ALU.add, [R_ex], [R_ex])
                    recip(ex2[:, :], ex2[:, :], [R_ex], [R_ex])
                    stt("dve", ex[:, :], ex[:, :], 1.0, ex2[:, :], ALU.add, ALU.mult, [R_ex], [R_ex])
                    red("dve", sm, ex[:, :], ALU.add, [R_ex], [R_ex])
                    recip(sm, sm, [R_ex], [R_ex])
                    ts1("dve", aff[:, j, :], ex[:, :], sm, ALU.mult, [R_ex], [R_aff])
                return f

            return dict(S=[cS(g) for g in range(4)], G=[cG(db) for db in range(8)], X1=[cX1(i) for i in range(4)],
                        X2a=[cX2a(i) for i in range(4)], X2b=[cX2b(i) for i in range(4)], XT=[cXT(i) for i in range(4)],
                        X2l=[cX2l(i) for i in range(4)])

        for j in range(3):
            load_xn(j)
        load_xr(0)
        load_xr(1)
        def run(lst):
            for f in lst:
                f()

        p1 = part1(0)
        p2 = part2(0)
        run(p1["AB"])
        for i in range(4):
            p1["Va"][i]()
            p1["U"][i]()
            p1["Vb"][i]()
        for g in range(4):
            p2["S"][g]()
        nopl = [lambda: None] * 4
        for sti in range(8):
            nxt = sti + 1 < 8
            p1n = part1(sti + 1) if nxt else None
            p2n = part2(sti + 1) if nxt else None
            for k in range(8):
                p2["G"][k]()
                if nxt:
                    p1n["AB"][k]()
            X1, Xa, Xb, T, Xl = p2["X1"], p2["X2a"], p2["X2b"], p2["XT"], p2["X2l"]
            Va = p1n["Va"] if nxt else nopl
            Vb = p1n["Vb"] if nxt else nopl
            for f in (X1[0], Va[0], T[0], X1[1], Vb[0], Xa[0], Va[1], T[1], X1[2], Xl[0], Vb[1], Xb[0], Xa[1], Va[2], T[2], X1[3],
                      Xl[1], Vb[2], Xb[1], Xa[2], Va[3], T[3], Xl[2], Vb[3], Xb[2], Xa[3], Xl[3], Xb[3]):
                f()
            if nxt:
                for g in range(4):
                    p1n["U"][g]()
                    p2n["S"][g]()
            p2 = p2n
        P.barrier()
        if stop == "C":
            finish()
            return nc
        o = 0
        affP = carve(o, [128, 512], F32); o += 2048
        junk = carve(o, [128, 512], BF16); o += 1024
        maskb = carve(o, [128, 512], BF16); o += 1024
        posS = carve(o, [128, 512], F32); o += 2048
        aS = carve(o, [128, 512], F32); o += 2048
        bS = carve(o, [128, 32, NE], F32); o += 2048
        m2 = carve(o, [128, 512], F32); o += 2048
        eqm = carve(o, [128, 512, 4], F32); o += 8192
        vals4 = carve(o, [128, 512, 4], F32); o += 8192
        OT = 149504
        ahb = carve(OT, [128, 512], BF16)
        A4 = carve(OT + 1024, [128, 512, 16], BF16)
        Bm = [carve(OT + 17408 + i * 8192, [128, 32, 128], BF16) for i in range(2)]
        resS = carve(OT + 33792, [128, 4, 4], F32)
        idxf = carve(OT + 33856, [128, 4], F32)
        assert OT + 33872 <= ARENA_W * 4
        thrS = carve(o, [128, NE], F32); o += 64
        diag = carve(o, [16, NE], F32); o += 64
        bis = carve(o, [128, 8], F32); o += 32
        PH_D_END = o
        R_affT = Res()
        R_b = Res()
        aff2 = aff[:, :, :].rearrange("p a b -> p (a b)")
        bk, br = nb()
        for q in range(4):
            mm(bk[:, q * 128:(q + 1) * 128], aff2[:, q * 128:(q + 1) * 128], identF, True, True, [R_aff] + CR, [br], q == 3)
        cp("act", affP[:, :], bk[:, :], [br], [R_affT])
        lo_, hi_, mid_, cnt_, flg_, d1_ = (bis[:, i:i + 1] for i in range(6))
        P.op("dve", lambda e: e.memset(bis[:, :], 0.0), [], [R_b])
        for it in range(24):
            hk = 2.0 ** -(it + 1)
            ts1("dve", mid_, lo_, hk, ALU.add, [R_b], [R_b])
            ts("dve", junk[:, :], affP[:, :], mid_, 0.0, ALU.is_ge, ALU.add, [R_b, R_affT], [R_b], accum_out=cnt_)
            bk, br = nb()
            mm(bk[:, 0:2], Bsum, bis[:, 3:5], True, True, [R_b] + CR, [br], True)
            ts("dve", flg_, bk[:, 0:1], CAP - 0.5, hk, ALU.is_gt, ALU.mult, [br, R_b], [R_b])
            tt("dve", lo_, lo_, flg_, ALU.add, [R_b], [R_b])
        ts1("dve", diag[:, :], identF[0:16, 0:16], bis[0:16, 0:1], ALU.mult, [R_b] + CR, [R_b])
        bk, br = nb()
        mm(bk[:, 0:NE], onesF[0:16, :], diag[:, :], True, True, [R_b] + CR, [br], True)
        R_m = Res()
        cp("dve", thrS[:, :], bk[:, 0:NE], [br], [R_m])
        if dbg:
            dma("sp", dbg_out["d_thr"], bis[0:16, 0:4], "dbg", [R_b], [])
            dma("sp", dbg_out["d_aff"], aff2, "dbg", [R_aff], [])
        mask3 = maskb[:, :].rearrange("p (a b) -> p a b", a=32)
        tt("dve", mask3, aff[:, :, :], thrS[:, :].unsqueeze(1).to_broadcast([128, 32, NE]), ALU.is_ge, [R_aff, R_m], [R_m])
        bk, br = nb()
        bk3 = bk[:, :].rearrange("p (a b) -> p a b", a=32)
        mm(bk[:, :], tri, maskb[:, :], True, False, [R_m] + CR, [br], False)
        for jp in range(31):
            mm(bk3[:, jp + 1:32, :], onesb, mask3[:, jp, :].unsqueeze(1).to_broadcast([128, 31 - jp, NE]), False, jp == 30,
               [R_m] + CR, [br], jp == 30)
        cp("dve", posS[:, :], bk[:, :], [br], [R_m])
        ts1("dve", aS[:, :], posS[:, :], 128.0, ALU.is_ge, [R_m], [R_m])
        stt("dve", aS[:, :], posS[:, :], 256.0, aS[:, :], ALU.is_ge, ALU.add, [R_m], [R_m])
        stt("dve", aS[:, :], posS[:, :], 384.0, aS[:, :], ALU.is_ge, ALU.add, [R_m], [R_m])
        bS2 = bS[:, :, :].rearrange("p a b -> p (a b)")
        stt("dve", bS2, aS[:, :], -128.0, posS[:, :], ALU.mult, ALU.add, [R_m], [R_m])
        ts1("dve", m2[:, :], posS[:, :], float(CAP), ALU.is_lt, [R_m], [R_m])
        tt("dve", m2[:, :], m2[:, :], maskb[:, :], ALU.mult, [R_m], [R_m])
        for ca in range(4):
            stt("dve", eqm[:, :, ca], aS[:, :], float(ca), m2[:, :], ALU.is_equal, ALU.mult, [R_m], [R_m])
        v44 = vals4[:, :, :].rearrange("p (a b) c -> p a b c", a=32)
        cp("dve", v44[:, :, :, 0], thi.unsqueeze(2).to_broadcast([128, 32, NE]), [R_m] + CR, [R_m])
        cp("dve", v44[:, :, :, 1], tlo.unsqueeze(2).to_broadcast([128, 32, NE]), [R_m] + CR, [R_m])
        cp("dve", ahb[:, :], aff2, [R_aff, R_m], [R_m])
        cp("dve", vals4[:, :, 2], ahb[:, :], [R_m], [R_m])
        tt("dve", vals4[:, :, 3], aff2, ahb[:, :], ALU.subtract, [R_aff, R_m], [R_m])
        A44 = A4[:, :, :].rearrange("p a (b c) -> p a b c", b=4)
        for ca in range(4):
            tt("dve", A44[:, :, ca, :], vals4[:, :, :], eqm[:, :, ca].unsqueeze(2).to_broadcast([128, 512, 4]), ALU.mult,
               [R_m], [R_m])
        A4e = A4[:, :, :].rearrange("p (a b) c -> p a b c", a=32)
        Bm_r = [Res() for _ in range(2)]
        R_res = Res()
        R_gi = Res()
        R_gie = [Res() for _ in range(NE)]
        bSb = ahb[:, :].rearrange("p (a b) -> p a b", a=32)
        cp("dve", ahb[:, :], bS2, [R_m], [R_m])

        def buildB(e_):
            s = e_ % 2
            tt("dve", Bm[s][:, :, :], iotaB.unsqueeze(1).to_broadcast([128, 32, 128]),
               bSb[:, :, e_].unsqueeze(2).to_broadcast([128, 32, 128]), ALU.is_equal, [R_m] + CR, [Bm_r[s]])

        o = 0
        WdB = [carve(o + i * 32768, [128, 16, D], BF16) for i in range(2)]; o += 65536
        hT = carve(o, [128, 16, 512], BF16); o += 16384
        xe = [carve(o + i * 8192, [128, 4, D], BF16) for i in range(2)]; o += 16384
        xeT = carve(o, [128, 8, 512], BF16); o += 8192
        ye = [carve(o + i * 4096, [128, D], F32) for i in range(2)]; o += 8192
        sa = [carve(o + i * 1024, [128, 512], BF16) for i in range(2)]; o += 2048
        NB = 4
        WgB = [carve(o + i * 4096, [128, 8, 256], BF16) for i in range(NB)]; o += NB * 4096
        WuB = [carve(o + i * 4096, [128, 8, 256], BF16) for i in range(NB)]; o += NB * 4096
        assert o <= OT
        WdB_r = [[Res() for _ in range(8)] for _ in range(2)]
        hT_r = [Res() for _ in range(16)]
        xe_r = [Res() for _ in range(2)]
        xeT_r = [Res() for _ in range(8)]
        ye_r = [Res() for _ in range(2)]
        sa_r = [Res() for _ in range(2)]
        WgB_r = [Res() for _ in range(NB)]
        WuB_r = [Res() for _ in range(NB)]
        NST = NE * 8
        PD = 3

        def w_load(s, parts="gud"):
            e_, st_ = divmod(s, 8)
            b = s % NB
            f0 = st_ * 256
            if "d" in parts:
                dma("pool", WdB[e_ % 2][:, st_ * 2:st_ * 2 + 2, :], wd[e_][f0:f0 + 256, :].rearrange("(a p) d -> p a d", p=128),
                    "wd%d" % (e_ % 2), [], [WdB_r[e_ % 2][st_]])
            if "g" not in parts:
                return
            dma("pool", WgB[b][:, :, :].rearrange("p c f -> p (c f)"), wg[s * 128:(s + 1) * 128, :], "wg%d" % b, [], [WgB_r[b]])
            dma("pool", WuB[b][:, :, :].rearrange("p c f -> p (c f)"), wu[s * 128:(s + 1) * 128, :], "wu%d" % b, [], [WuB_r[b]])

        def gather(e_):
            s = e_ % 2
            for ca in range(4):
                P.dma("pool", lambda e, s=s, ca=ca, e_=e_: e.indirect_dma_start(
                    out=xe[s][:, ca, :], out_offset=None, in_=xh2d,
                    in_offset=bass.IndirectOffsetOnAxis(ap=idxs[:, e_, ca:ca + 1], axis=0)),
                    "ga%d" % s, [R_gie[e_]] + xh2d_r, [xe_r[s]])

        assert PH_D_END <= 65536
        def idxM(e_):
            s = e_ % 2
            bk, br = nb()
            for j in range(32):
                mm(bk[:, 0:16], Bm[s][:, j, :], A4e[:, j, e_, :], j == 0, j == 31, [Bm_r[s], R_m], [br], j == 31)
            cp("act", resS[:, :, :].rearrange("p a b -> p (a b)"), bk[:, 0:16], [br], [R_res])
            stt("dve", idxf[:, :], resS[:, :, 0], 64.0, resS[:, :, 1], ALU.mult, ALU.add, [R_res], [R_res])
            cp("dve", idxs[:, e_, :], idxf[:, :], [R_res], [R_gie[e_]])
            tt("dve", gates[:, e_, :], resS[:, :, 2], resS[:, :, 3], ALU.add, [R_res], [R_gie[e_]])
            gather(e_)

        buildB(0)
        idxM(0)
        for s_ in range(PD):
            w_load(s_, "gu")
        P._deps("pool", [R_m, R_b, R_affT], [R_m, R_b, R_affT])

        if stop == "D":
            finish()
            return nc
        sc_prev = [None]
        sc_cur = [None]
        P.dsem("sc")
        for s in range(PD):
            w_load(s, "d")
        for e_ in range(NE):
            xs = e_ % 2
            for c in range(8):
                bk, br = nb()
                for ca in range(4):
                    mm(bk[:, ca * 128:(ca + 1) * 128], xe[xs][:, ca, c * 128:(c + 1) * 128], ident, True, True,
                       [xe_r[xs]] + CR, [br], ca == 3)
                cp("act" if c % 2 == 0 else "dve", xeT[:, c, :], bk[:, :], [br], [xeT_r[c]])
            if e_ + 1 < NE:
                buildB(e_ + 1)
            for st_ in range(8):
                s = e_ * 8 + st_
                b = s % NB
                if st_ == 2 and e_ + 1 < NE:
                    idxM(e_ + 1)
                if s + PD < NST:
                    w_load(s + PD)
                for h in range(2):
                    fb = st_ * 2 + h
                    bA, rA = nb()
                    for c in range(8):
                        mm(bA[:, :], WgB[b][:, c, h * 128:(h + 1) * 128], xeT[:, c, :], c == 0, c == 7,
                           [WgB_r[b], xeT_r[c]], [rA], c == 7)
                    bG, rG = nb()
                    for c in range(8):
                        mm(bG[:, :], WuB[b][:, c, h * 128:(h + 1) * 128], xeT[:, c, :], c == 0, c == 7,
                           [WuB_r[b], xeT_r[c]], [rG], c == 7)
                    q = fb % 2
                    act(sa[q][:, :], bA[:, :], AF.Silu, [rA], [sa_r[q]])
                    tt("dve", hT[:, fb, :], bG[:, :], sa[q][:, :], ALU.mult, [rG, sa_r[q]], [hT_r[fb]])
            for ca in range(4):
                ys = ca % 2
                for hf in range(2):
                    bk, br = nb()
                    for fb in range(16):
                        mm(bk[:, :], hT[:, fb, ca * 128:(ca + 1) * 128], WdB[e_ % 2][:, fb, hf * 512:(hf + 1) * 512], fb == 0, fb == 15,
                           [hT_r[fb]] + WdB_r[e_ % 2], [br], fb == 15)
                    ts1("dve", ye[ys][:, hf * 512:(hf + 1) * 512], bk[:, :], gates[:, e_, ca:ca + 1], ALU.mult,
                        [br, R_gie[e_]], [ye_r[ys]])
                if sc_prev[0] is not None:
                    P._wait("pool", "sc", sc_prev[0])
                tok = P.dma("pool", lambda e, ys=ys, ca=ca, e_=e_: e.indirect_dma_start(
                    out=acc, out_offset=bass.IndirectOffsetOnAxis(ap=idxs[:, e_, ca:ca + 1], axis=0),
                    in_=ye[ys][:, :], in_offset=None, compute_op=ALU.add),
                    "sc", [R_gie[e_], ye_r[ys]], [])
                sc_cur[0] = tok[1]
            sc_prev[0] = sc_cur[0]
            for r_ in acc_r:
                r_.w = ("sc", sc_cur[0])
                r_.r = []
        P.barrier()

        if dbg:
            R_d2 = Res()
            dbgi = carve(0, [128, 64], F32)
            cp("dve", dbgi[:, :], idxs[:, :, :].rearrange("p a b -> p (a b)"), R_gie, [R_d2])
            dma("sp", dbg_out["d_idx"], dbgi[:, :], "dbg", [R_d2], [])
            dma("sp", dbg_out["d_gate"], gates[:, :, :].rearrange("p a b -> p (a b)"), "dbg", R_gie, [])
        if dbg:
            P.barrier()
        if stop == "E":
            finish()
            return nc
        o = 0
        fgbc = carve(o, [128, D], F32); o += 4096
        NXF, NYF, PF = 6, 6, 5
        xf = [carve(o + i * 4096, [128, D], F32) for i in range(NXF)]; o += NXF * 4096
        yf = [carve(o + i * 4096, [128, D], F32) for i in range(NYF)]; o += NYF * 4096
        jf = carve(o, [128, D], BF16); o += 2048
        xf_r = [Res() for _ in range(NXF)]
        yf_r = [Res() for _ in range(NYF)]
        R_fg = Res()
        dma("sp", fgbc[:, :], fg.partition_broadcast(128), "fgc", [], [R_fg])

        def load_f(j):
            sl = j % NXF
            dma("sp", xf[sl][:, :], acc[j * 128:(j + 1) * 128, :], "xf%d" % sl, [acc_r[j]], [xf_r[sl]])

        for j in range(PF):
            load_f(j)
        last = []
        for j in range(32 + 2):
            if j < 32:
                sl = j % NXF
                ys = j % NYF
                col = (j % 8) * 2
                ss = small[:, col:col + 1]
                rs = small[:, col + 1:col + 2]
                R_s = Res()
                act(jf[:, :], xf[sl][:, :], AF.Square, [xf_r[sl]], [R_s], scale=1.0 / 32.0, accum_out=ss)
                act(ss, ss, AF.Identity, [R_s], [R_s], scale=1.0, bias=EPS)
                P.op("pool", lambda e, rs=rs, ss=ss: e.tensor_tensor(out=rs, in0=ss, in1=mhalf[:, 0:1], op=ALU.pow), [R_s] + CR, [R_s])
                stt("dve", yf[ys][:, :], xf[sl][:, :], rs, fgbc[:, :], ALU.mult, ALU.mult, [xf_r[sl], R_s, R_fg], [yf_r[ys]])
                if j + PF < 32:
                    load_f(j + PF)
            k = j - 2
            if 0 <= k < 32:
                ks = k % NYF
                t = dma("sp", out[k * 128:(k + 1) * 128, :], yf[ks][:, :], "ost%d" % (k % 2), [yf_r[ks]], [Res()])
                last.append(t)
        for t in last[-2:]:
            P._wait("sp", t[0], t[1])
        P.barrier()

        finish()
    return nc


_CACHE = {}


def _consts():
    if "c" in _CACHE:
        return _CACHE["c"]
    bf = ml_dtypes.bfloat16
    p = np.arange(128)
    ident = np.eye(128, dtype=np.float32)
    tri = (p[:, None] < p[None, :]).astype(np.float32)
    ones = np.ones((128, 128), np.float32)
    ang = 2.0 * np.pi * ((p[:, None] * p[None, :]) % 128) / 128.0
    Cc = np.cos(ang) / np.sqrt(128.0)
    Sc = np.sin(ang) / np.sqrt(128.0)
    iota = np.broadcast_to(p[None, :].astype(np.float32), (128, 128))
    cbf = np.concatenate([ident, tri, ones, Cc, Sc, iota], axis=1).astype(bf)
    t = 128 * np.arange(32)[None, :] + p[:, None]
    bsum = ((p[:, None] % 16) == (p[None, :] % 16)).astype(np.float32)
    cf = np.concatenate([ident, iota, (t // 64).astype(np.float32), (t % 64).astype(np.float32), bsum], axis=1).astype(np.float32)
    n = np.arange(2049, dtype=np.int64)
    ph = (n[:, None] * n[None, :]) % 4096
    a2 = 2.0 * np.pi * ph.astype(np.float64) / 4096.0
    Cs = np.cos(a2) / 64.0
    Ss = np.sin(a2) / 64.0
    dC = Cs[0:2048, 0:2048].astype(bf)
    dCr = Cs[2048:2049, 0:2048].astype(bf)
    col = np.zeros((17 * 128,), np.float64)
    col[0:2049] = Cs[:, 2048]
    c8 = np.zeros((128, 17, 2), np.float64)
    c8[:, :, 0] = col.reshape(17, 128).T
    dC8 = c8.reshape(128, 34).astype(bf)
    dS = Ss[0:2048, 0:2048].astype(bf)
    c = dict(cbf=np.ascontiguousarray(cbf), cf=np.ascontiguousarray(cf), dC=np.ascontiguousarray(dC),
             dCr=np.ascontiguousarray(dCr), dC8=np.ascontiguousarray(dC8), dS=np.ascontiguousarray(dS))
    _CACHE["c"] = c
    return c


def _in_maps(inputs, cores):
    f = lambda a: np.ascontiguousarray(np.asarray(a, dtype=np.float32))
    x = f(inputs["x"])

    def relay(w):
        w = np.asarray(w, dtype=np.float32).reshape(NE, 8, 128, 8, 256)
        return np.ascontiguousarray(np.transpose(w, (0, 3, 2, 1, 4))).reshape(NE * 8 * 128, 2048)

    shared = dict(
        w_in=f(inputs["w_in"][0]), w_fo=f(inputs["w_fourier_out"][0]), w_so=f(inputs["w_sgu_out"][0]),
        w_out=f(inputs["w_out"][0]), w_r=f(inputs["w_router"][0]), wg=relay(inputs["w_gate_e"][0]),
        wu=relay(inputs["w_up_e"][0]), wd=f(inputs["w_down_e"][0]), g1=f(inputs["norm1_g"][0]),
        g2=f(inputs["norm2_g"][0]), fg=f(inputs["final_g"]), lng=f(inputs["sgu_ln_g"][0]), lnb=f(inputs["sgu_ln_b"][0]),
        wsT=f(np.transpose(np.asarray(inputs["w_spatial"][0]), (2, 0, 1)).reshape(128, 512)),
        bs=f(np.asarray(inputs["b_spatial"][0]).reshape(1, 512)),
    )
    shared.update(_consts())
    maps = []
    for b in cores:
        m = dict(shared)
        m["x"] = np.ascontiguousarray(x[b])
        maps.append(m)
    return maps


def kernel(**inputs):
    if "nc" not in _CACHE:
        _CACHE["nc"] = build(False)
    nc = _CACHE["nc"]
    maps = _in_maps(inputs, list(range(8)))
    res = run_bass_kernel_spmd(nc, maps, core_ids=list(range(8)))
    return np.stack([np.asarray(r["out"], dtype=np.float32) for r in res.results], axis=0)
```

```python
from contextlib import ExitStack
import numpy as np
import ml_dtypes
import concourse.bass as bass
import concourse.mybir as mybir
from concourse.bass_utils import run_bass_kernel_spmd

F32 = mybir.dt.float32
BF16 = mybir.dt.bfloat16
I32 = mybir.dt.int32
ALU = mybir.AluOpType
AF = mybir.ActivationFunctionType
AX = mybir.AxisListType

ENGS = ("pe", "act", "dve", "pool", "sp")
EPS = 1e-6
NTOK = 4096
D = 1024
NE = 16
CAP = 512


class Res:
    __slots__ = ("w", "r", "name")

    def __init__(self, name=""):
        self.w = None
        self.r = []
        self.name = name


class Prog:
    def __init__(self, nc, stack):
        self.nc = nc
        self.stack = stack
        self.ops = {e: [] for e in ENGS}
        self.cnt = {e: 0 for e in ENGS}
        self.pending = {e: False for e in ENGS}
        self.sems = {}
        self.dcnt = {}
        self.seen = {e: {} for e in ENGS}
        for e in ENGS:
            self.sems[e] = stack.enter_context(nc.semaphore("s_" + e))

    def dsem(self, name):
        if name not in self.sems:
            self.sems[name] = self.stack.enter_context(self.nc.semaphore("d_" + name))
            self.dcnt[name] = 0
        return name

    def _wait(self, eng, k, v):
        if self.seen[eng].get(k, 0) < v:
            self.seen[eng][k] = v
            self.ops[eng].append(("wait", k, v))

    def _deps(self, eng, reads, writes):
        deps = {}

        def add(tok):
            if tok is None:
                return
            k, v = tok
            if k == eng and eng == "pe":
                return
            if deps.get(k, 0) < v:
                deps[k] = v

        for b in reads:
            add(b.w)
        for b in writes:
            add(b.w)
            for t in b.r:
                add(t)
        for k, v in deps.items():
            self._wait(eng, k, v)

    def _commit(self, tok, reads, writes):
        for b in reads:
            b.r.append(tok)
        for b in writes:
            b.w = tok
            b.r = []

    def op(self, eng, fn, reads=(), writes=(), inc=True):
        self._deps(eng, reads, writes)
        if inc:
            self.cnt[eng] += 1
            self.pending[eng] = False
            tok = (eng, self.cnt[eng])
            self.ops[eng].append(("op", fn, eng, 1))
        else:
            self.pending[eng] = True
            tok = (eng, self.cnt[eng] + 1)
            self.ops[eng].append(("op", fn, None, 0))
        self._commit(tok, reads, writes)
        return tok

    def dma(self, q, fn, sem, reads=(), writes=()):
        self.dsem(sem)
        self._deps(q, reads, writes)
        self.dcnt[sem] += 16
        tok = (sem, self.dcnt[sem])
        self.ops[q].append(("op", fn, sem, 16))
        self._commit(tok, reads, writes)
        return tok

    def barrier(self):
        assert not any(self.pending.values())
        for e in ENGS:
            for k in ENGS:
                if k != e and self.cnt[k] > 0:
                    self._wait(e, k, self.cnt[k])
            for k, v in self.dcnt.items():
                if v > 0:
                    self._wait(e, k, v)

    def flush_one(self, e, h):
        for o in self.ops[e]:
            if o[0] == "wait":
                h.wait_ge(self.sems[o[1]], o[2])
            else:
                ins = o[1](h)
                if o[2] is not None:
                    ins.then_inc(self.sems[o[2]], o[3])


class _Stop(Exception):
    pass


def build(dbg=False, stop=None):
    nc = bass.Bass("TRN2", target_bir_lowering=False)

    def din(name, shape, dt=F32):
        return nc.dram_tensor(name, shape, dt, kind="ExternalInput").ap()

    x = din("x", [NTOK, D])
    w_in = din("w_in", [D, 3584])
    wfo = din("w_fo", [512, D])
    wso = din("w_so", [512, D])
    wout = din("w_out", [D, D])
    wr = din("w_r", [D, NE])
    wg = din("wg", [NE * 8 * 128, 2048])
    wu = din("wu", [NE * 8 * 128, 2048])
    wd = din("wd", [NE, 2048, D])
    g1 = din("g1", [D])
    g2 = din("g2", [D])
    fg = din("fg", [D])
    lng = din("lng", [512])
    lnb = din("lnb", [512])
    wsT_d = din("wsT", [128, 512])
    bs_d = din("bs", [1, 512])
    cbf_d = din("cbf", [128, 768], BF16)
    cf_d = din("cf", [128, 448], F32)
    dC = din("dC", [2048, 2048], BF16)
    dCr = din("dCr", [1, 2048], BF16)
    dC8 = din("dC8", [128, 34], BF16)
    dS = din("dS", [2048, 2048], BF16)
    out = nc.dram_tensor("out", [NTOK, D], F32, kind="ExternalOutput").ap()
    acc = nc.dram_tensor("acc", [NTOK, D], F32).ap()
    xh2d = nc.dram_tensor("xh2d", [NTOK, D], BF16).ap()
    zfd = nc.dram_tensor("zfd", [128, 4 * NTOK], BF16).ap()
    dbg_out = {}
    if dbg:
        for nm, shp in (("d_zf", [128, 4 * 4096]), ("d_aff", [128, 512]), ("d_idx", [128, 64]), ("d_gate", [128, 64]),
                        ("d_thr", [16, 4])):
            dbg_out[nm] = nc.dram_tensor(nm, shp, F32, kind="ExternalOutput").ap()

    with ExitStack() as st:
        P = Prog(nc, st)
        def A(name, shape, dt):
            return nc.alloc_sbuf_tensor("sb_" + name, shape, dt)

        cbf = A("cbf", [128, 768], BF16)
        ident, tri, onesb, Cc, Sc, iotaB = (cbf[:, i * 128:(i + 1) * 128] for i in range(6))
        cf = A("cf", [128, 448], F32)
        identF, iotaF, thi, tlo, Bsum = cf[:, 0:128], cf[:, 128:256], cf[:, 256:288], cf[:, 288:320], cf[:, 320:448]
        g1bc = A("g1bc", [128, D], F32)
        g2bc = A("g2bc", [128, D], F32)
        lngbc = A("lngbc", [128, 512], F32)
        lnbbc = A("lnbbc", [128, 512], F32)
        wsTb = A("wsTb", [128, 512], BF16)
        bsb = A("bsb", [1, 512], BF16)
        wrb = A("wrb", [128, 8, NE], BF16)
        aff = A("aff", [128, 32, NE], F32)
        gates = A("gates", [128, NE, 4], F32)
        idxs = A("idxs", [128, NE, 4], I32)
        small = A("small", [128, 64], F32)
        onesF = A("onesF", [128, 128], F32)
        mhalf = A("mhalf", [128, 4], F32)
        ARENA_W = 47104
        arena = A("arena", [128, ARENA_W], F32)
        R_const = Res("const")

        def carve(off_b, shape, dt):
            n = int(np.prod(shape[1:]))
            esz = 2 if dt == BF16 else 4
            assert off_b % 4 == 0 and off_b + n * esz <= ARENA_W * 4, (off_b, shape)
            w0 = off_b // 4
            w1 = w0 + (n * esz + 3) // 4
            ap = arena[0:shape[0], w0:w1]
            if dt != F32:
                ap = ap.bitcast(dt)
            ap = ap[:, 0:n]
            if len(shape) == 3:
                ap = ap.rearrange("p (a b) -> p a b", a=shape[1])
            elif len(shape) == 4:
                ap = ap.rearrange("p (a b c) -> p a b c", a=shape[1], b=shape[2])
            return ap

        banks = [nc.alloc_psum_tensor("bank%d" % i, [128, 512], F32) for i in range(8)]
        bankR = [Res("bank%d" % i) for i in range(8)]
        bstate = {"i": 0}

        def nb():
            i = bstate["i"]
            bstate["i"] = (i + 1) % 8
            return banks[i], bankR[i]

        def mm(o, lhsT, rhs, start, stop, reads, writes, inc):
            P.op("pe", lambda e: e.matmul(o, lhsT, rhs, start=start, stop=stop), reads, writes, inc=inc)

        def act(o, i, func, reads, writes, **kw):
            P.op("act", lambda e: e.activation(out=o, in_=i, func=func, **kw), reads, writes)

        def tt(eng, o, a, b, op, reads, writes):
            P.op(eng, lambda e: e.tensor_tensor(out=o, in0=a, in1=b, op=op), reads, writes)

        def ts(eng, o, a, s1, s2, op0, op1, reads, writes, accum_out=None):
            if accum_out is None:
                P.op(eng, lambda e: e.tensor_scalar(out=o, in0=a, scalar1=s1, scalar2=s2, op0=op0, op1=op1), reads, writes)
            else:
                P.op(eng, lambda e: e.tensor_scalar(out=o, in0=a, scalar1=s1, scalar2=s2, op0=op0, op1=op1,
                                                    accum_out=accum_out), reads, writes)

        def ts1(eng, o, a, s1, op0, reads, writes):
            P.op(eng, lambda e: e.tensor_scalar(out=o, in0=a, scalar1=s1, scalar2=None, op0=op0), reads, writes)

        def stt(eng, o, a, s, b, op0, op1, reads, writes):
            P.op(eng, lambda e: e.scalar_tensor_tensor(out=o, in0=a, scalar=s, in1=b, op0=op0, op1=op1), reads, writes)

        def cp(eng, o, i, reads, writes):
            if eng == "act":
                P.op("act", lambda e: e.activation(out=o, in_=i, func=AF.Copy), reads, writes)
            else:
                P.op(eng, lambda e: e.tensor_copy(out=o, in_=i), reads, writes)

        def red(eng, o, i, op, reads, writes):
            P.op(eng, lambda e: e.tensor_reduce(out=o, in_=i, axis=AX.X, op=op), reads, writes)

        def recip(o, i, reads, writes):
            P.op("dve", lambda e: e.reciprocal(out=o, in_=i), reads, writes)

        def dma(q, o, i, sem, reads, writes):
            return P.dma(q, lambda e: e.dma_start(out=o, in_=i), sem, reads, writes)

        def finish():
            with nc.Block() as block:
                @block.sync
                def _(e):
                    P.flush_one("sp", e)

                @block.scalar
                def _(e):
                    P.flush_one("act", e)

                @block.vector
                def _(e):
                    P.flush_one("dve", e)

                @block.gpsimd
                def _(e):
                    P.flush_one("pool", e)

                @block.tensor
                def _(e):
                    P.flush_one("pe", e)

        dma("sp", cbf[:], cbf_d, "c0", [], [R_const])
        dma("sp", cf[:], cf_d, "c0", [], [R_const])
        dma("sp", g1bc[:], g1.partition_broadcast(128), "c0", [], [R_const])
        dma("sp", g2bc[:], g2.partition_broadcast(128), "c0", [], [R_const])
        dma("sp", lngbc[:], lng.partition_broadcast(128), "c0", [], [R_const])
        dma("sp", lnbbc[:], lnb.partition_broadcast(128), "c0", [], [R_const])
        R_c2 = Res("const2")
        dma("pool", wsTb[:], wsT_d, "c1", [], [R_c2])
        dma("pool", bsb[:], bs_d, "c1", [], [R_c2])
        dma("pool", wrb[:], wr.rearrange("(c p) e -> p c e", p=128), "c1", [], [R_c2])
        P.op("dve", lambda e: e.memset(onesF[:], 1.0), [], [R_c2])
        P.op("dve", lambda e: e.memset(mhalf[:], -0.5), [], [R_c2])
        CR = [R_const, R_c2]

        def rms_head(xt_ap, xt_res, junk_ap, col, **jkw):
            ss = small[:, col:col + 1]
            rs = small[:, col + 1:col + 2]
            R_s = Res("s")
            act(junk_ap, xt_ap, AF.Square, [xt_res], [R_s], scale=1.0 / 32.0, accum_out=ss, **jkw)
            act(ss, ss, AF.Identity, [R_s], [R_s], scale=1.0, bias=EPS)
            P.op("pool", lambda e: e.tensor_tensor(out=rs, in0=ss, in1=mhalf[:, 0:1], op=ALU.pow), [R_s] + CR, [R_s])
            return R_s

        def rms_tail(xt_ap, xt_res, xh_ap, xh_res, gbc, col, R_s):
            rs = small[:, col + 1:col + 2]
            stt("dve", xh_ap, xt_ap, rs, gbc, ALU.mult, ALU.mult, [xt_res, R_s] + CR, [xh_res])

        def transposes(src_ap, src_res, dst_ap, dst_res, i, evac_engs=("act", "dve")):
            for h in range(2):
                bk, br = nb()
                for q in range(4):
                    c = h * 4 + q
                    mm(bk[:, q * 128:(q + 1) * 128], src_ap[:, c * 128:(c + 1) * 128], ident, True, True,
                       [src_res] + CR, [br], q == 3)
                cp(evac_engs[h], dst_ap[:, h * 4:(h + 1) * 4, i * 128:(i + 1) * 128],
                   bk[:, :].rearrange("p (a b) -> p a b", a=4), [br], [dst_res])

        o = 0
        zT = carve(o, [128, 4, 4096], F32); o += 65536
        ze = carve(o, [128, 4, 2052], BF16); o += 4 * 2052 * 2
        zo = carve(o, [128, 4, 2052], BF16); o += 4 * 2052 * 2
        O_SMALL = o
        xt_s = [carve(o + i * 4096, [128, D], F32) for i in range(4)]; o += 4 * 4096
        xh_s = [carve(o + i * 2048, [128, D], BF16) for i in range(4)]; o += 4 * 2048
        xhT_s = [carve(o + i * 8192, [128, 8, 512], BF16) for i in range(2)]; o += 2 * 8192
        WfB = carve(o, [128, 8, 512], BF16); o += 8192
        junkA = carve(o, [128, D], BF16); o += 2048
        assert o <= ARENA_W * 4
        xt_r = [Res() for _ in range(4)]
        xh_r = [Res() for _ in range(4)]
        xhT_r = [Res() for _ in range(2)]
        R_wf = Res()
        R_zT = Res()
        w_in_v = w_in.rearrange("(c p) k -> p c k", p=128)
        dma("pool", WfB[:, :, :], w_in_v[:, :, 0:512], "wf", [], [R_wf])

        def load_x(j):
            slot = j % 4
            dma("sp", xt_s[slot][:, :], x[j * 128:(j + 1) * 128, :], "xt%d" % slot, [], [xt_r[slot]])

        rsA = {}

        def headA(j):
            sl = j % 4
            rsA[j] = rms_head(xt_s[sl][:, :], xt_r[sl], junkA[:, :], (j % 8) * 2)

        def tailA(j):
            sl, hs = j % 4, j % 4
            rms_tail(xt_s[sl][:, :], xt_r[sl], xh_s[hs][:, :], xh_r[hs], g1bc[:, :], (j % 8) * 2, rsA.pop(j))

        pendZ = []

        def transA(j):
            sti, i = divmod(j, 4)
            ts_ = sti % 2
            transposes(xh_s[j % 4], xh_r[j % 4], xhT_s[ts_], xhT_r[ts_], i)
            if i == 3:
                for g in range(4):
                    pendZ.append((j + 4 + g, sti, g))

        def zgroup(it, sti, g):
            ts_ = sti % 2
            bk, br = nb()
            for c in range(8):
                mm(bk[:, :], WfB[:, c, g * 128:(g + 1) * 128], xhT_s[ts_][:, c, :], c == 0, c == 7,
                   [R_wf, xhT_r[ts_]], [br], c == 7)
            deferredA.append((it + 1, "act" if g % 2 == 0 else "dve", zT[:, g, sti * 512:(sti + 1) * 512], bk, br))

        for j in range(3):
            load_x(j)
        deferredA = []
        for j in range(44):
            if j < 32:
                headA(j)
            if 0 <= j - 1 < 32:
                tailA(j - 1)
            if j + 3 < 32:
                load_x(j + 3)
            while deferredA and deferredA[0][0] <= j:
                _, eng_, dst_, bk_, br_ = deferredA.pop(0)
                cp(eng_, dst_, bk_[:, :], [br_], [R_zT])
            if 0 <= j - 4 < 32:
                transA(j - 4)
            while pendZ and pendZ[0][0] <= j:
                _, sti_, g_ = pendZ.pop(0)
                zgroup(j, sti_, g_)
        assert not deferredA and not pendZ
        R_ze = Res()
        R_zo = Res()
        tt("dve", ze[:, :, 1:2048], zT[:, :, 1:2048], zT[:, :, 4095:2048:-1], ALU.add, [R_zT], [R_ze])
        cp("dve", ze[:, :, 0:1], zT[:, :, 0:1], [R_zT], [R_ze])
        cp("dve", ze[:, :, 2048:2049], zT[:, :, 2048:2049], [R_zT], [R_ze])
        tt("dve", zo[:, :, 1:2048], zT[:, :, 1:2048], zT[:, :, 4095:2048:-1], ALU.subtract, [R_zT], [R_zo])
        P.op("dve", lambda e: e.memset(zo[:, :, 0:1], 0.0), [], [R_zo])
        P.barrier()

        if stop == "A":
            finish()
            return nc
        o = O_SMALL
        Y = carve(o, [128, 17, 2, 512], BF16); o += 17 * 2 * 512 * 2
        dftC = [carve(o + i * 8704, [128, 17, 256], BF16) for i in range(2)]; o += 2 * 8704
        dftS = [carve(o + i * 8192, [128, 16, 256], BF16) for i in range(2)]; o += 2 * 8192
        tmpB = [carve(o + i * 1024, [128, 256], F32) for i in range(2)]; o += 2048
        c8 = carve(o, [128, 17, 2], BF16); o += 68
        O_WOUT = 169984
        assert o <= O_WOUT
        zfT = carve(0, [128, 4, 4096], BF16)
        O_W2 = 32768
        Win2 = carve(O_W2, [128, 8, 3072], BF16)
        WfoB = carve(O_W2 + 49152, [128, 4, D], BF16)
        WsoB = carve(O_W2 + 57344, [128, 4, D], BF16)
        assert O_W2 + 65536 <= O_SMALL
        WoutB = carve(O_WOUT, [128, 8, D], BF16)
        R_Y = [Res() for _ in range(17)]
        R_zf = Res()
        dft_r = [Res() for _ in range(2)]
        tmpB_r = [Res() for _ in range(2)]
        R_c8 = Res()
        R_w2 = Res()
        dC_v = dC.rearrange("(t p) k -> p t k", p=128)
        dS_v = dS.rearrange("(t p) k -> p t k", p=128)

        def load_dft(kb):
            s = kb % 2
            k0 = kb * 256
            dma("sp", dftC[s][:, 0:16, :], dC_v[:, :, k0:k0 + 256], "dft%d" % s, [], [dft_r[s]])
            dma("sp", dftC[s][0:1, 16, :], dCr[0:1, k0:k0 + 256], "dft%d" % s, [], [dft_r[s]])
            dma("sp", dftS[s][:, :, :], dS_v[:, :, k0:k0 + 256], "dft%d" % s, [], [dft_r[s]])

        load_dft(0)
        load_dft(1)
        dma("sp", c8[:, :, :], dC8.rearrange("p (t w) -> p t w", w=2), "c8", [], [R_c8])
        for nt in range(17):
            rows = 128 if nt < 16 else 1
            bk, br = nb()
            for g in range(4):
                mm(bk[0:rows, g * 128:(g + 1) * 128], ze[:, g, nt * 128:nt * 128 + rows], Cc, True, True,
                   [R_ze] + CR, [br], g == 3)
            cp("act", Y[0:rows, nt, 0, :], bk[0:rows, :], [br], [R_Y[nt]])
            if nt < 16:
                bk, br = nb()
                for g in range(4):
                    mm(bk[:, g * 128:(g + 1) * 128], zo[:, g, nt * 128:(nt + 1) * 128], Sc, True, True,
                       [R_zo] + CR, [br], g == 3)
                cp("dve", Y[:, nt, 1, :], bk[:, :], [br], [R_Y[nt]])
        def prefetch_w2():
            for c in range(8):
                for h in range(2):
                    dma("pool", Win2[:, c, h * 1536:(h + 1) * 1536], w_in_v[:, c, 512 + h * 1536:512 + (h + 1) * 1536], "w2",
                        [], [R_w2, R_ze, R_zo])
            dma("pool", WfoB[:, :, :], wfo.rearrange("(c p) k -> p c k", p=128), "w2", [], [R_w2, R_ze, R_zo])
            dma("pool", WsoB[:, :, :], wso.rearrange("(c p) k -> p c k", p=128), "w2", [], [R_w2, R_ze, R_zo])
            dma("pool", WoutB[:, :, :], wout.rearrange("(c p) k -> p c k", p=128), "w2", [], [R_w2])

        for kb in range(8):
            s = kb % 2
            k0 = kb * 256
            for g in range(4):
                bA, rA = nb()
                for nt in range(17):
                    rows = 128 if nt < 16 else 1
                    mm(bA[:, 0:256], Y[0:rows, nt, 0, g * 128:(g + 1) * 128], dftC[s][0:rows, nt, :], nt == 0, nt == 16,
                       [R_Y[nt], dft_r[s]], [rA], nt == 16)
                bB, rB = nb()
                for nt in range(16):
                    mm(bB[:, 0:256], Y[:, nt, 1, g * 128:(g + 1) * 128], dftS[s][:, nt, :], nt == 0, nt == 15,
                       [R_Y[nt], dft_r[s]], [rB], nt == 15)
                tb = (kb * 4 + g) % 2
                cp("act", tmpB[tb][:, :], bB[:, 0:256], [rB], [tmpB_r[tb]])
                tt("dve", zfT[:, g, k0:k0 + 256], bA[:, 0:256], tmpB[tb][:, :], ALU.subtract, [rA, tmpB_r[tb]], [R_zf])
                lo = 1 if kb == 0 else 0
                tt("dve", zfT[:, g, NTOK - k0 - lo:NTOK - k0 - 256:-1], bA[:, lo:256], tmpB[tb][:, lo:256], ALU.add,
                   [rA, tmpB_r[tb]], [R_zf])
            if kb + 2 < 8:
                load_dft(kb + 2)
            if kb == 1:
                prefetch_w2()
        for g in range(4):
            bA, rA = nb()
            for nt in range(17):
                rows = 128 if nt < 16 else 1
                mm(bA[:, 0:2], Y[0:rows, nt, 0, g * 128:(g + 1) * 128], c8[0:rows, nt, :], nt == 0, nt == 16,
                   [R_Y[nt], R_c8], [rA], nt == 16)
            cp("act", zfT[:, g, 2048:2049], bA[:, 0:1], [rA], [R_zf])
        zf_flat = zfT[:, :, :].rearrange("p a b -> p (a b)")
        dma("sp", zfd, zf_flat, "zfw", [R_zf], [])
        if dbg:
            zdbg = carve(O_SMALL, [128, 4 * 4096], F32)
            R_d = Res()
            P.barrier()
            cp("dve", zdbg[:, :], zf_flat, [R_zf], [R_d])
            dma("sp", dbg_out["d_zf"], zdbg[:, :], "dbg", [R_d], [])
        P.barrier()

        if stop == "B":
            finish()
            return nc
        zfd_v = zfd.rearrange("p (a b) -> p a b", a=4)
        o = 0
        zfs = [carve(o + i * 4096, [128, 4, 512], BF16) for i in range(2)]; o += 8192
        xn = [carve(o + i * 4096, [128, D], F32) for i in range(3)]; o += 12288
        xr = [carve(o + i * 4096, [128, D], F32) for i in range(2)]; o += 8192
        xhc = [carve(o + i * 2048, [128, D], BF16) for i in range(2)]; o += 4096
        assert o <= 32768
        o = O_SMALL
        xhT2 = [carve(o + i * 8192, [128, 8, 512], BF16) for i in range(2)]; o += 16384
        vn42 = [carve(o + i * 4096, [128, 4, 512], BF16) for i in range(2)]; o += 8192
        ug2 = [carve(o + i * 4096, [128, 4, 512], BF16) for i in range(2)]; o += 8192
        sT = carve(o, [128, 4, 512], BF16); o += 4096
        sfs = [carve(o + i * 1024, [128, 512], BF16) for i in range(4)]; o += 4096
        t12 = [carve(o + i * 2048, [128, 512], F32) for i in range(2)]; o += 4096
        vgb = carve(o, [128, 4, 128], F32); o += 2048
        sqj = carve(o, [128, 512], BF16)
        junk8 = arena[:, o // 4:o // 4 + 256].bitcast(mybir.dt.float8e4)
        o += 1024
        mergedT = carve(o, [128, 8, 512], BF16); o += 8192
        x1t_s = [carve(o + i * 4096, [128, D], F32) for i in range(2)]; o += 8192
        xh2_s = [carve(o + i * 2048, [128, D], BF16) for i in range(2)]; o += 4096
        xh2T = carve(o, [128, 8, 128], BF16); o += 2048
        ex = carve(o, [128, NE], F32); o += 64
        ex2 = carve(o, [128, NE], F32); o += 64
        assert o <= O_WOUT, o
        zfs_r = [Res() for _ in range(2)]
        xn_r = [Res() for _ in range(3)]
        xr_r = [Res() for _ in range(2)]
        xhc_r = [Res() for _ in range(2)]
        xhT2_r = [Res() for _ in range(2)]
        vn_r = [[Res() for _ in range(4)] for _ in range(2)]
        ug_r = [[Res() for _ in range(4)] for _ in range(2)]
        sT_r = [Res() for _ in range(4)]
        sfs_r = [Res() for _ in range(4)]
        t12_r = [Res() for _ in range(2)]
        R_vg = Res()
        mg_r = [Res() for _ in range(8)]
        R_x1s = [Res(), Res()]
        R_xh2s = [Res(), Res()]
        R_xh2T = Res()
        R_ex = Res()
        R_aff = Res()
        acc_r = [Res() for _ in range(32)]
        xh2d_r = [Res() for _ in range(32)]
        UO, VO, GFO, GSO = 0, 512, 1024, 2048

        def load_xn(j):
            sl = j % 3
            dma("sp", xn[sl][:, :], x[j * 128:(j + 1) * 128, :], "xt%d" % sl, [], [xn_r[sl]])

        def load_xr(j):
            sl = j % 2
            dma("sp", xr[sl][:, :], x[j * 128:(j + 1) * 128, :], "xr%d" % sl, [], [xr_r[sl]])

        def part1(st):
            s2 = st % 2
            n0 = st * 512

            rsC = {}

            def hA(i):
                def f():
                    j = st * 4 + i
                    if i == 0:
                        dma("sp", zfs[s2][:, :, :], zfd_v[:, :, n0:n0 + 512], "zfs%d" % s2, [], [zfs_r[s2]])
                    rsC[i] = rms_head(xn[j % 3][:, :], xn_r[j % 3], junk8, 16 + (j % 8) * 2, saturate=False)
                return f

            def tA(i):
                def f():
                    j = st * 4 + i
                    rms_tail(xn[j % 3][:, :], xn_r[j % 3], xhc[i % 2][:, :], xhc_r[i % 2], g1bc[:, :], 16 + (j % 8) * 2, rsC.pop(i))
                    if j + 3 < 32:
                        load_xn(j + 3)
                return f

            def cB(i):
                def f():
                    transposes(xhc[i % 2], xhc_r[i % 2], xhT2[s2], xhT2_r[s2], i)
                return f

            def cVa(i):
                def f():
                    bk, br = nb()
                    for c in range(8):
                        mm(bk[:, :], xhT2[s2][:, c, i * 128:(i + 1) * 128], Win2[:, c, VO:VO + 512], c == 0, c == 7,
                           [xhT2_r[s2], R_w2], [br], c == 7)
                    vg2 = vgb[:, :, :].rearrange("p a b -> p (a b)")
                    act(vg2, bk[:, :], AF.Gelu_apprx_tanh, [br], [R_vg])
                    s1 = small[:, 40:44]
                    s2c = small[:, 44:48]
                    red("dve", s1, vgb[:, :, :], ALU.add, [R_vg], [R_vg])
                    ts1("dve", s1, s1, 1.0 / 128, ALU.mult, [R_vg], [R_vg])
                    tt("dve", vgb[:, :, :], vgb[:, :, :], s1.unsqueeze(2).to_broadcast([128, 4, 128]), ALU.subtract, [R_vg], [R_vg])
                    for g in range(4):
                        act(sqj[:, g * 128:(g + 1) * 128], vgb[:, g, :], AF.Square, [R_vg], [R_vg], scale=float(128.0 ** -0.5),
                            accum_out=s2c[:, g:g + 1])
                    act(s2c, s2c, AF.Identity, [R_vg], [R_vg], scale=1.0, bias=EPS)
                    P.op("pool", lambda e: e.tensor_tensor(out=s2c, in0=s2c, in1=mhalf[:, 0:4], op=ALU.pow), [R_vg] + CR, [R_vg])
                return f

            def cVb(i):
                def f():
                    vg2 = vgb[:, :, :].rearrange("p a b -> p (a b)")
                    s2c = small[:, 44:48]
                    for g in range(4):
                        stt("dve", vgb[:, g, :], vgb[:, g, :], s2c[:, g:g + 1], lngbc[:, g * 128:(g + 1) * 128], ALU.mult, ALU.mult,
                            [R_vg] + CR, [R_vg])
                    tt("dve", vn42[s2][:, i, :], vg2, lnbbc[:, :], ALU.add, [R_vg] + CR, [vn_r[s2][i]])
                return f

            def cU(cb):
                def f():
                    bk, br = nb()
                    for c in range(8):
                        mm(bk[:, :], Win2[:, c, UO + cb * 128:UO + (cb + 1) * 128], xhT2[s2][:, c, :], c == 0, c == 7,
                           [xhT2_r[s2], R_w2], [br], c == 7)
                    act(ug2[s2][:, cb, :], bk[:, :], AF.Gelu_apprx_tanh, [br], [ug_r[s2][cb]])
                return f

            def seq(*fs):
                def f():
                    for g_ in fs:
                        g_()
                return f

            nop = lambda: None
            AB = [hA(0), seq(hA(1), tA(0)), seq(hA(2), tA(1), cB(0)), seq(hA(3), tA(2), cB(1)), seq(tA(3), cB(2)), cB(3), nop, nop]
            return dict(AB=AB, Va=[cVa(i) for i in range(4)], Vb=[cVb(i) for i in range(4)], U=[cU(cb) for cb in range(4)])

        def part2(st):
            s2 = st % 2
            n0 = st * 512

            def cS(g):
                def f():
                    bk, br = nb()
                    for i in range(4):
                        mm(bk[:, i * 128:(i + 1) * 128], vn42[s2][:, i, g * 128:(g + 1) * 128], wsTb[:, g * 128:(g + 1) * 128],
                           True, False, [vn_r[s2][i]] + CR, [br], False)
                        mm(bk[:, i * 128:(i + 1) * 128], onesb[0:1, :], bsb[0:1, g * 128:(g + 1) * 128],
                           False, True, CR, [br], i == 3)
                    tt("dve", sT[:, g, :], bk[:, :], ug2[s2][:, g, :], ALU.mult, [br, ug_r[s2][g]], [sT_r[g]])
                return f

            def cG(db):
                def f():
                    bgf, rgf = nb()
                    for c in range(8):
                        mm(bgf[:, :], Win2[:, c, GFO + db * 128:GFO + (db + 1) * 128], xhT2[s2][:, c, :], c == 0, c == 7,
                           [xhT2_r[s2], R_w2], [rgf], c == 7)
                    bgs, rgs = nb()
                    for c in range(8):
                        mm(bgs[:, :], Win2[:, c, GSO + db * 128:GSO + (db + 1) * 128], xhT2[s2][:, c, :], c == 0, c == 7,
                           [xhT2_r[s2], R_w2], [rgs], c == 7)
                    byf, ryf = nb()
                    for g in range(4):
                        mm(byf[:, :], WfoB[:, g, db * 128:(db + 1) * 128], zfs[s2][:, g, :], g == 0, g == 3,
                           [zfs_r[s2], R_w2], [ryf], g == 3)
                    bys, rys = nb()
                    for g in range(4):
                        mm(bys[:, :], WsoB[:, g, db * 128:(db + 1) * 128], sT[:, g, :], g == 0, g == 3,
                           [sT_r[g], R_w2], [rys], g == 3)
                    q = (db % 2) * 2
                    act(sfs[q][:, :], bgf[:, :], AF.Sigmoid, [rgf], [sfs_r[q]])
                    act(sfs[q + 1][:, :], bgs[:, :], AF.Sigmoid, [rgs], [sfs_r[q + 1]])
                    tt("dve", t12[0][:, :], byf[:, :], sfs[q][:, :], ALU.mult, [ryf, sfs_r[q]], [t12_r[0]])
                    tt("dve", t12[1][:, :], bys[:, :], sfs[q + 1][:, :], ALU.mult, [rys, sfs_r[q + 1]], [t12_r[1]])
                    tt("pool", mergedT[:, db, :], t12[0][:, :], t12[1][:, :], ALU.add, [t12_r[0], t12_r[1]], [mg_r[db]])
                return f

            rsX = {}

            def cX1(i):
                def f():
                    j = st * 4 + i
                    sl = j % 2
                    x1t, xh2, R_x1, R_xh2 = x1t_s[sl], xh2_s[sl], R_x1s[sl], R_xh2s[sl]
                    for hf in range(2):
                        bk, br = nb()
                        for c in range(8):
                            mm(bk[:, :], mergedT[:, c, i * 128:(i + 1) * 128], WoutB[:, c, hf * 512:(hf + 1) * 512], c == 0, c == 7,
                               [mg_r[c], R_w2], [br], c == 7)
                        tt("dve", x1t[:, hf * 512:(hf + 1) * 512], bk[:, :], xr[sl][:, hf * 512:(hf + 1) * 512], ALU.add,
                           [br, xr_r[sl]], [R_x1])
                    if j + 2 < 32:
                        load_xr(j + 2)
                    dma("sp", acc[j * 128:(j + 1) * 128, :], x1t[:, :], "accw", [R_x1], [acc_r[j]])
                    rsX[i] = rms_head(x1t[:, :], R_x1, junk8, 32 + (j % 4) * 2, saturate=False)
                return f

            def cXT(i):
                def f():
                    j = st * 4 + i
                    sl = j % 2
                    x1t, xh2, R_x1, R_xh2 = x1t_s[sl], xh2_s[sl], R_x1s[sl], R_xh2s[sl]
                    rms_tail(x1t[:, :], R_x1, xh2[:, :], R_xh2, g2bc[:, :], 32 + (j % 4) * 2, rsX.pop(i))
                    dma("sp", xh2d[j * 128:(j + 1) * 128, :], xh2[:, :], "xh2w", [R_xh2], [xh2d_r[j]])
                return f

            def cX2a(i):
                def f():
                    j = st * 4 + i
                    sl = j % 2
                    x1t, xh2, R_x1, R_xh2 = x1t_s[sl], xh2_s[sl], R_x1s[sl], R_xh2s[sl]
                    transposes(xh2, R_xh2, xh2T, R_xh2T, 0)
                return f

            def cX2l(i):
                def f():
                    bk, br = nb()
                    for c in range(8):
                        mm(bk[:, 0:NE], xh2T[:, c, :], wrb[:, c, :], c == 0, c == 7, [R_xh2T] + CR, [br], c == 7)
                    mx = small[:, 48:49]
                    red("dve", mx, bk[:, 0:NE], ALU.max, [br], [R_ex])
                    ts1("dve", mx, mx, -0.5, ALU.mult, [R_ex], [R_ex])
                    act(ex[:, :], bk[:, 0:NE], AF.Tanh, [br, R_ex], [R_ex], bias=mx, scale=0.5)
                return f

            def cX2b(i):
                def f():
                    j = st * 4 + i
                    sm = small[:, 49:50]
                    ts("dve", ex2[:, :], ex[:, :], -1.0, 1.0, ALU.mult, ALU.add, [R_ex], [R_ex])
                    recip(ex2[:, :], ex2[:, :], [R_ex], [R_ex])
                    stt("dve", ex[:, :], ex[:, :], 1.0, ex2[:, :], ALU.add, ALU.mult, [R_ex], [R_ex])
                    red("dve", sm, ex[:, :], ALU.add, [R_ex], [R_ex])
                    recip(sm, sm, [R_ex], [R_ex])
                    ts1("dve", aff[:, j, :], ex[:, :], sm, ALU.mult, [R_ex], [R_aff])
                return f

            return dict(S=[cS(g) for g in range(4)], G=[cG(db) for db in range(8)], X1=[cX1(i) for i in range(4)],
                        X2a=[cX2a(i) for i in range(4)], X2b=[cX2b(i) for i in range(4)], XT=[cXT(i) for i in range(4)],
                        X2l=[cX2l(i) for i in range(4)])

        for j in range(3):
            load_xn(j)
        load_xr(0)
        load_xr(1)
        def run(lst):
            for f in lst:
                f()

        p1 = part1(0)
        p2 = part2(0)
        run(p1["AB"])
        for i in range(4):
            p1["Va"][i]()
            p1["U"][i]()
            p1["Vb"][i]()
        for g in range(4):
            p2["S"][g]()
        nopl = [lambda: None] * 4
        for sti in range(8):
            nxt = sti + 1 < 8
            p1n = part1(sti + 1) if nxt else None
            p2n = part2(sti + 1) if nxt else None
            for k in range(8):
                p2["G"][k]()
                if nxt:
                    p1n["AB"][k]()
            X1, Xa, Xb, T, Xl = p2["X1"], p2["X2a"], p2["X2b"], p2["XT"], p2["X2l"]
            Va = p1n["Va"] if nxt else nopl
            Vb = p1n["Vb"] if nxt else nopl
            for f in (X1[0], Va[0], T[0], X1[1], Vb[0], Xa[0], Va[1], T[1], X1[2], Xl[0], Vb[1], Xb[0], Xa[1], Va[2], T[2], X1[3],
                      Xl[1], Vb[2], Xb[1], Xa[2], Va[3], T[3], Xl[2], Vb[3], Xb[2], Xa[3], Xl[3], Xb[3]):
                f()
            if nxt:
                for g in range(4):
                    p1n["U"][g]()
                    p2n["S"][g]()
            p2 = p2n
        P.barrier()
        if stop == "C":
            finish()
            return nc
        o = 0
        affP = carve(o, [128, 512], F32); o += 2048
        junk = carve(o, [128, 512], BF16); o += 1024
        maskb = carve(o, [128, 512], BF16); o += 1024
        posS = carve(o, [128, 512], F32); o += 2048
        aS = carve(o, [128, 512], F32); o += 2048
        bS = carve(o, [128, 32, NE], F32); o += 2048
        m2 = carve(o, [128, 512], F32); o += 2048
        eqm = carve(o, [128, 512, 4], F32); o += 8192
        vals4 = carve(o, [128, 512, 4], F32); o += 8192
        OT = 149504
        ahb = carve(OT, [128, 512], BF16)
        A4 = carve(OT + 1024, [128, 512, 16], BF16)
        Bm = [carve(OT + 17408 + i * 8192, [128, 32, 128], BF16) for i in range(2)]
        resS = carve(OT + 33792, [128, 4, 4], F32)
        idxf = carve(OT + 33856, [128, 4], F32)
        assert OT + 33872 <= ARENA_W * 4
        thrS = carve(o, [128, NE], F32); o += 64
        diag = carve(o, [16, NE], F32); o += 64
        bis = carve(o, [128, 8], F32); o += 32
        PH_D_END = o
        R_affT = Res()
        R_b = Res()
        aff2 = aff[:, :, :].rearrange("p a b -> p (a b)")
        bk, br = nb()
        for q in range(4):
            mm(bk[:, q * 128:(q + 1) * 128], aff2[:, q * 128:(q + 1) * 128], identF, True, True, [R_aff] + CR, [br], q == 3)
        cp("act", affP[:, :], bk[:, :], [br], [R_affT])
        lo_, hi_, mid_, cnt_, flg_, d1_ = (bis[:, i:i + 1] for i in range(6))
        P.op("dve", lambda e: e.memset(bis[:, :], 0.0), [], [R_b])
        for it in range(24):
            hk = 2.0 ** -(it + 1)
            ts1("dve", mid_, lo_, hk, ALU.add, [R_b], [R_b])
            ts("dve", junk[:, :], affP[:, :], mid_, 0.0, ALU.is_ge, ALU.add, [R_b, R_affT], [R_b], accum_out=cnt_)
            bk, br = nb()
            mm(bk[:, 0:2], Bsum, bis[:, 3:5], True, True, [R_b] + CR, [br], True)
            ts("dve", flg_, bk[:, 0:1], CAP - 0.5, hk, ALU.is_gt, ALU.mult, [br, R_b], [R_b])
            tt("dve", lo_, lo_, flg_, ALU.add, [R_b], [R_b])
        ts1("dve", diag[:, :], identF[0:16, 0:16], bis[0:16, 0:1], ALU.mult, [R_b] + CR, [R_b])
        bk, br = nb()
        mm(bk[:, 0:NE], onesF[0:16, :], diag[:, :], True, True, [R_b] + CR, [br], True)
        R_m = Res()
        cp("dve", thrS[:, :], bk[:, 0:NE], [br], [R_m])
        if dbg:
            dma("sp", dbg_out["d_thr"], bis[0:16, 0:4], "dbg", [R_b], [])
            dma("sp", dbg_out["d_aff"], aff2, "dbg", [R_aff], [])
        mask3 = maskb[:, :].rearrange("p (a b) -> p a b", a=32)
        tt("dve", mask3, aff[:, :, :], thrS[:, :].unsqueeze(1).to_broadcast([128, 32, NE]), ALU.is_ge, [R_aff, R_m], [R_m])
        bk, br = nb()
        bk3 = bk[:, :].rearrange("p (a b) -> p a b", a=32)
        mm(bk[:, :], tri, maskb[:, :], True, False, [R_m] + CR, [br], False)
        for jp in range(31):
            mm(bk3[:, jp + 1:32, :], onesb, mask3[:, jp, :].unsqueeze(1).to_broadcast([128, 31 - jp, NE]), False, jp == 30,
               [R_m] + CR, [br], jp == 30)
        cp("dve", posS[:, :], bk[:, :], [br], [R_m])
        ts1("dve", aS[:, :], posS[:, :], 128.0, ALU.is_ge, [R_m], [R_m])
        stt("dve", aS[:, :], posS[:, :], 256.0, aS[:, :], ALU.is_ge, ALU.add, [R_m], [R_m])
        stt("dve", aS[:, :], posS[:, :], 384.0, aS[:, :], ALU.is_ge, ALU.add, [R_m], [R_m])
        bS2 = bS[:, :, :].rearrange("p a b -> p (a b)")
        stt("dve", bS2, aS[:, :], -128.0, posS[:, :], ALU.mult, ALU.add, [R_m], [R_m])
        ts1("dve", m2[:, :], posS[:, :], float(CAP), ALU.is_lt, [R_m], [R_m])
        tt("dve", m2[:, :], m2[:, :], maskb[:, :], ALU.mult, [R_m], [R_m])
        for ca in range(4):
            stt("dve", eqm[:, :, ca], aS[:, :], float(ca), m2[:, :], ALU.is_equal, ALU.mult, [R_m], [R_m])
        v44 = vals4[:, :, :].rearrange("p (a b) c -> p a b c", a=32)
        cp("dve", v44[:, :, :, 0], thi.unsqueeze(2).to_broadcast([128, 32, NE]), [R_m] + CR, [R_m])
        cp("dve", v44[:, :, :, 1], tlo.unsqueeze(2).to_broadcast([128, 32, NE]), [R_m] + CR, [R_m])
        cp("dve", ahb[:, :], aff2, [R_aff, R_m], [R_m])
        cp("dve", vals4[:, :, 2], ahb[:, :], [R_m], [R_m])
        tt("dve", vals4[:, :, 3], aff2, ahb[:, :], ALU.subtract, [R_aff, R_m], [R_m])
        A44 = A4[:, :, :].rearrange("p a (b c) -> p a b c", b=4)
        for ca in range(4):
            tt("dve", A44[:, :, ca, :], vals4[:, :, :], eqm[:, :, ca].unsqueeze(2).to_broadcast([128, 512, 4]), ALU.mult,
               [R_m], [R_m])
        A4e = A4[:, :, :].rearrange("p (a b) c -> p a b c", a=32)
        Bm_r = [Res() for _ in range(2)]
        R_res = Res()
        R_gi = Res()
        R_gie = [Res() for _ in range(NE)]
        bSb = ahb[:, :].rearrange("p (a b) -> p a b", a=32)
        cp("dve", ahb[:, :], bS2, [R_m], [R_m])

        def buildB(e_):
            s = e_ % 2
            tt("dve", Bm[s][:, :, :], iotaB.unsqueeze(1).to_broadcast([128, 32, 128]),
               bSb[:, :, e_].unsqueeze(2).to_broadcast([128, 32, 128]), ALU.is_equal, [R_m] + CR, [Bm_r[s]])

        o = 0
        WdB = [carve(o + i * 32768, [128, 16, D], BF16) for i in range(2)]; o += 65536
        hT = carve(o, [128, 16, 512], BF16); o += 16384
        xe = [carve(o + i * 8192, [128, 4, D], BF16) for i in range(2)]; o += 16384
        xeT = carve(o, [128, 8, 512], BF16); o += 8192
        ye = [carve(o + i * 4096, [128, D], F32) for i in range(2)]; o += 8192
        sa = [carve(o + i * 1024, [128, 512], BF16) for i in range(2)]; o += 2048
        NB = 4
        WgB = [carve(o + i * 4096, [128, 8, 256], BF16) for i in range(NB)]; o += NB * 4096
        WuB = [carve(o + i * 4096, [128, 8, 256], BF16) for i in range(NB)]; o += NB * 4096
        assert o <= OT
        WdB_r = [[Res() for _ in range(8)] for _ in range(2)]
        hT_r = [Res() for _ in range(16)]
        xe_r = [Res() for _ in range(2)]
        xeT_r = [Res() for _ in range(8)]
        ye_r = [Res() for _ in range(2)]
        sa_r = [Res() for _ in range(2)]
        WgB_r = [Res() for _ in range(NB)]
        WuB_r = [Res() for _ in range(NB)]
        NST = NE * 8
        PD = 3

        def w_load(s, parts="gud"):
            e_, st_ = divmod(s, 8)
            b = s % NB
            f0 = st_ * 256
            if "d" in parts:
                dma("pool", WdB[e_ % 2][:, st_ * 2:st_ * 2 + 2, :], wd[e_][f0:f0 + 256, :].rearrange("(a p) d -> p a d", p=128),
                    "wd%d" % (e_ % 2), [], [WdB_r[e_ % 2][st_]])
            if "g" not in parts:
                return
            dma("pool", WgB[b][:, :, :].rearrange("p c f -> p (c f)"), wg[s * 128:(s + 1) * 128, :], "wg%d" % b, [], [WgB_r[b]])
            dma("pool", WuB[b][:, :, :].rearrange("p c f -> p (c f)"), wu[s * 128:(s + 1) * 128, :], "wu%d" % b, [], [WuB_r[b]])

        def gather(e_):
            s = e_ % 2
            for ca in range(4):
                P.dma("pool", lambda e, s=s, ca=ca, e_=e_: e.indirect_dma_start(
                    out=xe[s][:, ca, :], out_offset=None, in_=xh2d,
                    in_offset=bass.IndirectOffsetOnAxis(ap=idxs[:, e_, ca:ca + 1], axis=0)),
                    "ga%d" % s, [R_gie[e_]] + xh2d_r, [xe_r[s]])

        assert PH_D_END <= 65536
        def idxM(e_):
            s = e_ % 2
            bk, br = nb()
            for j in range(32):
                mm(bk[:, 0:16], Bm[s][:, j, :], A4e[:, j, e_, :], j == 0, j == 31, [Bm_r[s], R_m], [br], j == 31)
            cp("act", resS[:, :, :].rearrange("p a b -> p (a b)"), bk[:, 0:16], [br], [R_res])
            stt("dve", idxf[:, :], resS[:, :, 0], 64.0, resS[:, :, 1], ALU.mult, ALU.add, [R_res], [R_res])
            cp("dve", idxs[:, e_, :], idxf[:, :], [R_res], [R_gie[e_]])
            tt("dve", gates[:, e_, :], resS[:, :, 2], resS[:, :, 3], ALU.add, [R_res], [R_gie[e_]])

        buildB(0)
        idxM(0)
        gather(0)
        for s_ in range(PD):
            w_load(s_, "gu")
        P._deps("pool", [R_m, R_b, R_affT], [R_m, R_b, R_affT])

        if stop == "D":
            finish()
            return nc
        sc_prev = [None]
        sc_cur = [None]
        P.dsem("sc")
        for s in range(PD):
            w_load(s, "d")
        for e_ in range(NE):
            xs = e_ % 2
            for c in range(8):
                bk, br = nb()
                for ca in range(4):
                    mm(bk[:, ca * 128:(ca + 1) * 128], xe[xs][:, ca, c * 128:(c + 1) * 128], ident, True, True,
                       [xe_r[xs]] + CR, [br], ca == 3)
                cp("act" if c % 2 == 0 else "dve", xeT[:, c, :], bk[:, :], [br], [xeT_r[c]])
            if e_ + 1 < NE:
                buildB(e_ + 1)
            for st_ in range(8):
                s = e_ * 8 + st_
                b = s % NB
                if st_ == 2 and e_ + 1 < NE:
                    idxM(e_ + 1)
                if s + PD < NST:
                    w_load(s + PD)
                if st_ == 5 and e_ + 1 < NE:
                    gather(e_ + 1)
                for h in range(2):
                    fb = st_ * 2 + h
                    bA, rA = nb()
                    for c in range(8):
                        mm(bA[:, :], WgB[b][:, c, h * 128:(h + 1) * 128], xeT[:, c, :], c == 0, c == 7,
                           [WgB_r[b], xeT_r[c]], [rA], c == 7)
                    bG, rG = nb()
                    for c in range(8):
                        mm(bG[:, :], WuB[b][:, c, h * 128:(h + 1) * 128], xeT[:, c, :], c == 0, c == 7,
                           [WuB_r[b], xeT_r[c]], [rG], c == 7)
                    q = fb % 2
                    act(sa[q][:, :], bA[:, :], AF.Silu, [rA], [sa_r[q]])
                    tt("dve", hT[:, fb, :], bG[:, :], sa[q][:, :], ALU.mult, [rG, sa_r[q]], [hT_r[fb]])
            for ca in range(4):
                ys = ca % 2
                for hf in range(2):
                    bk, br = nb()
                    for fb in range(16):
                        mm(bk[:, :], hT[:, fb, ca * 128:(ca + 1) * 128], WdB[e_ % 2][:, fb, hf * 512:(hf + 1) * 512], fb == 0, fb == 15,
                           [hT_r[fb]] + WdB_r[e_ % 2], [br], fb == 15)
                    ts1("dve", ye[ys][:, hf * 512:(hf + 1) * 512], bk[:, :], gates[:, e_, ca:ca + 1], ALU.mult,
                        [br, R_gie[e_]], [ye_r[ys]])
                if sc_prev[0] is not None:
                    P._wait("pool", "sc", sc_prev[0])
                tok = P.dma("pool", lambda e, ys=ys, ca=ca, e_=e_: e.indirect_dma_start(
                    out=acc, out_offset=bass.IndirectOffsetOnAxis(ap=idxs[:, e_, ca:ca + 1], axis=0),
                    in_=ye[ys][:, :], in_offset=None, compute_op=ALU.add),
                    "sc", [R_gie[e_], ye_r[ys]], [])
                sc_cur[0] = tok[1]
            sc_prev[0] = sc_cur[0]
            for r_ in acc_r:
                r_.w = ("sc", sc_cur[0])
                r_.r = []
        P.barrier()

        if dbg:
            R_d2 = Res()
            dbgi = carve(0, [128, 64], F32)
            cp("dve", dbgi[:, :], idxs[:, :, :].rearrange("p a b -> p (a b)"), R_gie, [R_d2])
            dma("sp", dbg_out["d_idx"], dbgi[:, :], "dbg", [R_d2], [])
            dma("sp", dbg_out["d_gate"], gates[:, :, :].rearrange("p a b -> p (a b)"), "dbg", R_gie, [])
        if dbg:
            P.barrier()
        if stop == "E":
            finish()
            return nc
        o = 0
        fgbc = carve(o, [128, D], F32); o += 4096
        NXF, NYF, PF = 6, 6, 5
        xf = [carve(o + i * 4096, [128, D], F32) for i in range(NXF)]; o += NXF * 4096
        yf = [carve(o + i * 4096, [128, D], F32) for i in range(NYF)]; o += NYF * 4096
        jf = carve(o, [128, D], BF16); o += 2048
        xf_r = [Res() for _ in range(NXF)]
        yf_r = [Res() for _ in range(NYF)]
        R_fg = Res()
        dma("sp", fgbc[:, :], fg.partition_broadcast(128), "fgc", [], [R_fg])

        def load_f(j):
            sl = j % NXF
            dma("sp", xf[sl][:, :], acc[j * 128:(j + 1) * 128, :], "xf%d" % sl, [acc_r[j]], [xf_r[sl]])

        for j in range(PF):
            load_f(j)
        last = []
        for j in range(32 + 2):
            if j < 32:
                sl = j % NXF
                ys = j % NYF
                col = (j % 8) * 2
                ss = small[:, col:col + 1]
                rs = small[:, col + 1:col + 2]
                R_s = Res()
                act(jf[:, :], xf[sl][:, :], AF.Square, [xf_r[sl]], [R_s], scale=1.0 / 32.0, accum_out=ss)
                act(ss, ss, AF.Identity, [R_s], [R_s], scale=1.0, bias=EPS)
                P.op("pool", lambda e, rs=rs, ss=ss: e.tensor_tensor(out=rs, in0=ss, in1=mhalf[:, 0:1], op=ALU.pow), [R_s] + CR, [R_s])
                stt("dve", yf[ys][:, :], xf[sl][:, :], rs, fgbc[:, :], ALU.mult, ALU.mult, [xf_r[sl], R_s, R_fg], [yf_r[ys]])
                if j + PF < 32:
                    load_f(j + PF)
            k = j - 2
            if 0 <= k < 32:
                ks = k % NYF
                t = dma("sp", out[k * 128:(k + 1) * 128, :], yf[ks][:, :], "ost%d" % (k % 2), [yf_r[ks]], [Res()])
                last.append(t)
        for t in last[-2:]:
            P._wait("sp", t[0], t[1])
        P.barrier()

        finish()
    return nc


_CACHE = {}


def _consts():
    if "c" in _CACHE:
        return _CACHE["c"]
    bf = ml_dtypes.bfloat16
    p = np.arange(128)
    ident = np.eye(128, dtype=np.float32)
    tri = (p[:, None] < p[None, :]).astype(np.float32)
    ones = np.ones((128, 128), np.float32)
    ang = 2.0 * np.pi * ((p[:, None] * p[None, :]) % 128) / 128.0
    Cc = np.cos(ang) / np.sqrt(128.0)
    Sc = np.sin(ang) / np.sqrt(128.0)
    iota = np.broadcast_to(p[None, :].astype(np.float32), (128, 128))
    cbf = np.concatenate([ident, tri, ones, Cc, Sc, iota], axis=1).astype(bf)
    t = 128 * np.arange(32)[None, :] + p[:, None]
    bsum = ((p[:, None] % 16) == (p[None, :] % 16)).astype(np.float32)
    cf = np.concatenate([ident, iota, (t // 64).astype(np.float32), (t % 64).astype(np.float32), bsum], axis=1).astype(np.float32)
    n = np.arange(2049, dtype=np.int64)
    ph = (n[:, None] * n[None, :]) % 4096
    a2 = 2.0 * np.pi * ph.astype(np.float64) / 4096.0
    Cs = np.cos(a2) / 64.0
    Ss = np.sin(a2) / 64.0
    dC = Cs[0:2048, 0:2048].astype(bf)
    dCr = Cs[2048:2049, 0:2048].astype(bf)
    col = np.zeros((17 * 128,), np.float64)
    col[0:2049] = Cs[:, 2048]
    c8 = np.zeros((128, 17, 2), np.float64)
    c8[:, :, 0] = col.reshape(17, 128).T
    dC8 = c8.reshape(128, 34).astype(bf)
    dS = Ss[0:2048, 0:2048].astype(bf)
    c = dict(cbf=np.ascontiguousarray(cbf), cf=np.ascontiguousarray(cf), dC=np.ascontiguousarray(dC),
             dCr=np.ascontiguousarray(dCr), dC8=np.ascontiguousarray(dC8), dS=np.ascontiguousarray(dS))
    _CACHE["c"] = c
    return c


def _in_maps(inputs, cores):
    f = lambda a: np.ascontiguousarray(np.asarray(a, dtype=np.float32))
    x = f(inputs["x"])

    def relay(w):
        w = np.asarray(w, dtype=np.float32).reshape(NE, 8, 128, 8, 256)
        return np.ascontiguousarray(np.transpose(w, (0, 3, 2, 1, 4))).reshape(NE * 8 * 128, 2048)

    shared = dict(
        w_in=f(inputs["w_in"][0]), w_fo=f(inputs["w_fourier_out"][0]), w_so=f(inputs["w_sgu_out"][0]),
        w_out=f(inputs["w_out"][0]), w_r=f(inputs["w_router"][0]), wg=relay(inputs["w_gate_e"][0]),
        wu=relay(inputs["w_up_e"][0]), wd=f(inputs["w_down_e"][0]), g1=f(inputs["norm1_g"][0]),
        g2=f(inputs["norm2_g"][0]), fg=f(inputs["final_g"]), lng=f(inputs["sgu_ln_g"][0]), lnb=f(inputs["sgu_ln_b"][0]),
        wsT=f(np.transpose(np.asarray(inputs["w_spatial"][0]), (2, 0, 1)).reshape(128, 512)),
        bs=f(np.asarray(inputs["b_spatial"][0]).reshape(1, 512)),
    )
    shared.update(_consts())
    maps = []
    for b in cores:
        m = dict(shared)
        m["x"] = np.ascontiguousarray(x[b])
        maps.append(m)
    return maps


def kernel(**inputs):
    if "nc" not in _CACHE:
        _CACHE["nc"] = build(False)
    nc = _CACHE["nc"]
    maps = _in_maps(inputs, list(range(8)))
    res = run_bass_kernel_spmd(nc, maps, core_ids=list(range(8)))
    return np.stack([np.asarray(r["out"], dtype=np.float32) for r in res.results], axis=0)
```

```python
from contextlib import ExitStack
import numpy as np
import ml_dtypes
import concourse.bass as bass
import concourse.mybir as mybir
from concourse.bass_utils import run_bass_kernel_spmd

F32 = mybir.dt.float32
BF16 = mybir.dt.bfloat16
I32 = mybir.dt.int32
ALU = mybir.AluOpType
AF = mybir.ActivationFunctionType
AX = mybir.AxisListType

ENGS = ("pe", "act", "dve", "pool", "sp")
EPS = 1e-6
NTOK = 4096
D = 1024
NE = 16
CAP = 512


class Res:
    __slots__ = ("w", "r", "name")

    def __init__(self, name=""):
        self.w = None
        self.r = []
        self.name = name


class Prog:
    def __init__(self, nc, stack):
        self.nc = nc
        self.stack = stack
        self.ops = {e: [] for e in ENGS}
        self.cnt = {e: 0 for e in ENGS}
        self.pending = {e: False for e in ENGS}
        self.sems = {}
        self.dcnt = {}
        self.seen = {e: {} for e in ENGS}
        for e in ENGS:
            self.sems[e] = stack.enter_context(nc.semaphore("s_" + e))

    def dsem(self, name):
        if name not in self.sems:
            self.sems[name] = self.stack.enter_context(self.nc.semaphore("d_" + name))
            self.dcnt[name] = 0
        return name

    def _wait(self, eng, k, v):
        if self.seen[eng].get(k, 0) < v:
            self.seen[eng][k] = v
            self.ops[eng].append(("wait", k, v))

    def _deps(self, eng, reads, writes):
        deps = {}

        def add(tok):
            if tok is None:
                return
            k, v = tok
            if k == eng and eng == "pe":
                return
            if deps.get(k, 0) < v:
                deps[k] = v

        for b in reads:
            add(b.w)
        for b in writes:
            add(b.w)
            for t in b.r:
                add(t)
        for k, v in deps.items():
            self._wait(eng, k, v)

    def _commit(self, tok, reads, writes):
        for b in reads:
            b.r.append(tok)
        for b in writes:
            b.w = tok
            b.r = []

    def op(self, eng, fn, reads=(), writes=(), inc=True):
        self._deps(eng, reads, writes)
        if inc:
            self.cnt[eng] += 1
            self.pending[eng] = False
            tok = (eng, self.cnt[eng])
            self.ops[eng].append(("op", fn, eng, 1))
        else:
            self.pending[eng] = True
            tok = (eng, self.cnt[eng] + 1)
            self.ops[eng].append(("op", fn, None, 0))
        self._commit(tok, reads, writes)
        return tok

    def dma(self, q, fn, sem, reads=(), writes=()):
        self.dsem(sem)
        self._deps(q, reads, writes)
        self.dcnt[sem] += 16
        tok = (sem, self.dcnt[sem])
        self.ops[q].append(("op", fn, sem, 16))
        self._commit(tok, reads, writes)
        return tok

    def barrier(self):
        assert not any(self.pending.values())
        for e in ENGS:
            for k in ENGS:
                if k != e and self.cnt[k] > 0:
                    self._wait(e, k, self.cnt[k])
            for k, v in self.dcnt.items():
                if v > 0:
                    self._wait(e, k, v)

    def flush_one(self, e, h):
        for o in self.ops[e]:
            if o[0] == "wait":
                h.wait_ge(self.sems[o[1]], o[2])
            else:
                ins = o[1](h)
                if o[2] is not None:
                    ins.then_inc(self.sems[o[2]], o[3])


class _Stop(Exception):
    pass


def build(dbg=False, stop=None):
    nc = bass.Bass("TRN2", target_bir_lowering=False)

    def din(name, shape, dt=F32):
        return nc.dram_tensor(name, shape, dt, kind="ExternalInput").ap()

    x = din("x", [NTOK, D])
    w_in = din("w_in", [D, 3584])
    wfo = din("w_fo", [512, D])
    wso = din("w_so", [512, D])
    wout = din("w_out", [D, D])
    wr = din("w_r", [D, NE])
    wg = din("wg", [NE * 8 * 128, 2048])
    wu = din("wu", [NE * 8 * 128, 2048])
    wd = din("wd", [NE, 2048, D])
    g1 = din("g1", [D])
    g2 = din("g2", [D])
    fg = din("fg", [D])
    lng = din("lng", [512])
    lnb = din("lnb", [512])
    wsT_d = din("wsT", [128, 512])
    bs_d = din("bs", [1, 512])
    cbf_d = din("cbf", [128, 768], BF16)
    cf_d = din("cf", [128, 448], F32)
    dC = din("dC", [2048, 2048], BF16)
    dCr = din("dCr", [1, 2048], BF16)
    dC8 = din("dC8", [128, 34], BF16)
    dS = din("dS", [2048, 2048], BF16)
    out = nc.dram_tensor("out", [NTOK, D], F32, kind="ExternalOutput").ap()
    acc = nc.dram_tensor("acc", [NTOK, D], F32).ap()
    xh2d = nc.dram_tensor("xh2d", [NTOK, D], BF16).ap()
    zfd = nc.dram_tensor("zfd", [128, 4 * NTOK], BF16).ap()
    dbg_out = {}
    if dbg:
        for nm, shp in (("d_zf", [128, 4 * 4096]), ("d_aff", [128, 512]), ("d_idx", [128, 64]), ("d_gate", [128, 64]),
                        ("d_thr", [16, 4])):
            dbg_out[nm] = nc.dram_tensor(nm, shp, F32, kind="ExternalOutput").ap()

    with ExitStack() as st:
        P = Prog(nc, st)
        def A(name, shape, dt):
            return nc.alloc_sbuf_tensor("sb_" + name, shape, dt)

        cbf = A("cbf", [128, 768], BF16)
        ident, tri, onesb, Cc, Sc, iotaB = (cbf[:, i * 128:(i + 1) * 128] for i in range(6))
        cf = A("cf", [128, 448], F32)
        identF, iotaF, thi, tlo, Bsum = cf[:, 0:128], cf[:, 128:256], cf[:, 256:288], cf[:, 288:320], cf[:, 320:448]
        g1bc = A("g1bc", [128, D], F32)
        g2bc = A("g2bc", [128, D], F32)
        lngbc = A("lngbc", [128, 512], F32)
        lnbbc = A("lnbbc", [128, 512], F32)
        wsTb = A("wsTb", [128, 512], BF16)
        bsb = A("bsb", [1, 512], BF16)
        wrb = A("wrb", [128, 8, NE], BF16)
        aff = A("aff", [128, 32, NE], F32)
        gates = A("gates", [128, NE, 4], F32)
        idxs = A("idxs", [128, NE, 4], I32)
        small = A("small", [128, 64], F32)
        onesF = A("onesF", [128, 128], F32)
        mhalf = A("mhalf", [128, 4], F32)
        ARENA_W = 47104
        arena = A("arena", [128, ARENA_W], F32)
        R_const = Res("const")

        def carve(off_b, shape, dt):
            n = int(np.prod(shape[1:]))
            esz = 2 if dt == BF16 else 4
            assert off_b % 4 == 0 and off_b + n * esz <= ARENA_W * 4, (off_b, shape)
            w0 = off_b // 4
            w1 = w0 + (n * esz + 3) // 4
            ap = arena[0:shape[0], w0:w1]
            if dt != F32:
                ap = ap.bitcast(dt)
            ap = ap[:, 0:n]
            if len(shape) == 3:
                ap = ap.rearrange("p (a b) -> p a b", a=shape[1])
            elif len(shape) == 4:
                ap = ap.rearrange("p (a b c) -> p a b c", a=shape[1], b=shape[2])
            return ap

        banks = [nc.alloc_psum_tensor("bank%d" % i, [128, 512], F32) for i in range(8)]
        bankR = [Res("bank%d" % i) for i in range(8)]
        bstate = {"i": 0}

        def nb():
            i = bstate["i"]
            bstate["i"] = (i + 1) % 8
            return banks[i], bankR[i]

        def mm(o, lhsT, rhs, start, stop, reads, writes, inc):
            P.op("pe", lambda e: e.matmul(o, lhsT, rhs, start=start, stop=stop), reads, writes, inc=inc)

        def act(o, i, func, reads, writes, **kw):
            P.op("act", lambda e: e.activation(out=o, in_=i, func=func, **kw), reads, writes)

        def tt(eng, o, a, b, op, reads, writes):
            P.op(eng, lambda e: e.tensor_tensor(out=o, in0=a, in1=b, op=op), reads, writes)

        def ts(eng, o, a, s1, s2, op0, op1, reads, writes, accum_out=None):
            if accum_out is None:
                P.op(eng, lambda e: e.tensor_scalar(out=o, in0=a, scalar1=s1, scalar2=s2, op0=op0, op1=op1), reads, writes)
            else:
                P.op(eng, lambda e: e.tensor_scalar(out=o, in0=a, scalar1=s1, scalar2=s2, op0=op0, op1=op1,
                                                    accum_out=accum_out), reads, writes)

        def ts1(eng, o, a, s1, op0, reads, writes):
            P.op(eng, lambda e: e.tensor_scalar(out=o, in0=a, scalar1=s1, scalar2=None, op0=op0), reads, writes)

        def stt(eng, o, a, s, b, op0, op1, reads, writes):
            P.op(eng, lambda e: e.scalar_tensor_tensor(out=o, in0=a, scalar=s, in1=b, op0=op0, op1=op1), reads, writes)

        def cp(eng, o, i, reads, writes):
            if eng == "act":
                P.op("act", lambda e: e.activation(out=o, in_=i, func=AF.Copy), reads, writes)
            else:
                P.op(eng, lambda e: e.tensor_copy(out=o, in_=i), reads, writes)

        def red(eng, o, i, op, reads, writes):
            P.op(eng, lambda e: e.tensor_reduce(out=o, in_=i, axis=AX.X, op=op), reads, writes)

        def recip(o, i, reads, writes):
            P.op("dve", lambda e: e.reciprocal(out=o, in_=i), reads, writes)

        def dma(q, o, i, sem, reads, writes):
            return P.dma(q, lambda e: e.dma_start(out=o, in_=i), sem, reads, writes)

        def finish():
            with nc.Block() as block:
                @block.sync
                def _(e):
                    P.flush_one("sp", e)

                @block.scalar
                def _(e):
                    P.flush_one("act", e)

                @block.vector
                def _(e):
                    P.flush_one("dve", e)

                @block.gpsimd
                def _(e):
                    P.flush_one("pool", e)

                @block.tensor
                def _(e):
                    P.flush_one("pe", e)

        dma("sp", cbf[:], cbf_d, "c0", [], [R_const])
        dma("sp", cf[:], cf_d, "c0", [], [R_const])
        dma("sp", g1bc[:], g1.partition_broadcast(128), "c0", [], [R_const])
        dma("sp", g2bc[:], g2.partition_broadcast(128), "c0", [], [R_const])
        dma("sp", lngbc[:], lng.partition_broadcast(128), "c0", [], [R_const])
        dma("sp", lnbbc[:], lnb.partition_broadcast(128), "c0", [], [R_const])
        R_c2 = Res("const2")
        dma("pool", wsTb[:], wsT_d, "c1", [], [R_c2])
        dma("pool", bsb[:], bs_d, "c1", [], [R_c2])
        dma("pool", wrb[:], wr.rearrange("(c p) e -> p c e", p=128), "c1", [], [R_c2])
        P.op("dve", lambda e: e.memset(onesF[:], 1.0), [], [R_c2])
        P.op("dve", lambda e: e.memset(mhalf[:], -0.5), [], [R_c2])
        CR = [R_const, R_c2]

        def rms_head(xt_ap, xt_res, junk_ap, col, **jkw):
            ss = small[:, col:col + 1]
            rs = small[:, col + 1:col + 2]
            R_s = Res("s")
            act(junk_ap, xt_ap, AF.Square, [xt_res], [R_s], scale=1.0 / 32.0, accum_out=ss, **jkw)
            act(ss, ss, AF.Identity, [R_s], [R_s], scale=1.0, bias=EPS)
            P.op("pool", lambda e: e.tensor_tensor(out=rs, in0=ss, in1=mhalf[:, 0:1], op=ALU.pow), [R_s] + CR, [R_s])
            return R_s

        def rms_tail(xt_ap, xt_res, xh_ap, xh_res, gbc, col, R_s):
            rs = small[:, col + 1:col + 2]
            stt("dve", xh_ap, xt_ap, rs, gbc, ALU.mult, ALU.mult, [xt_res, R_s] + CR, [xh_res])

        def transposes(src_ap, src_res, dst_ap, dst_res, i, evac_engs=("act", "dve")):
            for h in range(2):
                bk, br = nb()
                for q in range(4):
                    c = h * 4 + q
                    mm(bk[:, q * 128:(q + 1) * 128], src_ap[:, c * 128:(c + 1) * 128], ident, True, True,
                       [src_res] + CR, [br], q == 3)
                cp(evac_engs[h], dst_ap[:, h * 4:(h + 1) * 4, i * 128:(i + 1) * 128],
                   bk[:, :].rearrange("p (a b) -> p a b", a=4), [br], [dst_res])

        o = 0
        zT = carve(o, [128, 4, 4096], F32); o += 65536
        ze = carve(o, [128, 4, 2052], BF16); o += 4 * 2052 * 2
        zo = carve(o, [128, 4, 2052], BF16); o += 4 * 2052 * 2
        O_SMALL = o
        xt_s = [carve(o + i * 4096, [128, D], F32) for i in range(4)]; o += 4 * 4096
        xh_s = [carve(o + i * 2048, [128, D], BF16) for i in range(4)]; o += 4 * 2048
        xhT_s = [carve(o + i * 8192, [128, 8, 512], BF16) for i in range(2)]; o += 2 * 8192
        WfB = carve(o, [128, 8, 512], BF16); o += 8192
        junkA = carve(o, [128, D], BF16); o += 2048
        assert o <= ARENA_W * 4
        xt_r = [Res() for _ in range(4)]
        xh_r = [Res() for _ in range(4)]
        xhT_r = [Res() for _ in range(2)]
        R_wf = Res()
        R_zT = Res()
        w_in_v = w_in.rearrange("(c p) k -> p c k", p=128)
        dma("pool", WfB[:, :, :], w_in_v[:, :, 0:512], "wf", [], [R_wf])

        def load_x(j):
            slot = j % 4
            dma("sp", xt_s[slot][:, :], x[j * 128:(j + 1) * 128, :], "xt%d" % slot, [], [xt_r[slot]])

        rsA = {}

        def headA(j):
            sl = j % 4
            rsA[j] = rms_head(xt_s[sl][:, :], xt_r[sl], junkA[:, :], (j % 8) * 2)

        def tailA(j):
            sl, hs = j % 4, j % 4
            rms_tail(xt_s[sl][:, :], xt_r[sl], xh_s[hs][:, :], xh_r[hs], g1bc[:, :], (j % 8) * 2, rsA.pop(j))

        pendZ = []

        def transA(j):
            sti, i = divmod(j, 4)
            ts_ = sti % 2
            transposes(xh_s[j % 4], xh_r[j % 4], xhT_s[ts_], xhT_r[ts_], i)
            if i == 3:
                for g in range(4):
                    pendZ.append((j + 4 + g, sti, g))

        def zgroup(it, sti, g):
            ts_ = sti % 2
            bk, br = nb()
            for c in range(8):
                mm(bk[:, :], WfB[:, c, g * 128:(g + 1) * 128], xhT_s[ts_][:, c, :], c == 0, c == 7,
                   [R_wf, xhT_r[ts_]], [br], c == 7)
            deferredA.append((it + 1, "act" if g % 2 == 0 else "dve", zT[:, g, sti * 512:(sti + 1) * 512], bk, br))

        for j in range(3):
            load_x(j)
        deferredA = []
        for j in range(44):
            if j < 32:
                headA(j)
            if 0 <= j - 1 < 32:
                tailA(j - 1)
            if j + 3 < 32:
                load_x(j + 3)
            while deferredA and deferredA[0][0] <= j:
                _, eng_, dst_, bk_, br_ = deferredA.pop(0)
                cp(eng_, dst_, bk_[:, :], [br_], [R_zT])
            if 0 <= j - 4 < 32:
                transA(j - 4)
            while pendZ and pendZ[0][0] <= j:
                _, sti_, g_ = pendZ.pop(0)
                zgroup(j, sti_, g_)
        assert not deferredA and not pendZ
        R_ze = Res()
        R_zo = Res()
        tt("dve", ze[:, :, 1:2048], zT[:, :, 1:2048], zT[:, :, 4095:2048:-1], ALU.add, [R_zT], [R_ze])
        cp("dve", ze[:, :, 0:1], zT[:, :, 0:1], [R_zT], [R_ze])
        cp("dve", ze[:, :, 2048:2049], zT[:, :, 2048:2049], [R_zT], [R_ze])
        tt("dve", zo[:, :, 1:2048], zT[:, :, 1:2048], zT[:, :, 4095:2048:-1], ALU.subtract, [R_zT], [R_zo])
        P.op("dve", lambda e: e.memset(zo[:, :, 0:1], 0.0), [], [R_zo])
        P.barrier()

        if stop == "A":
            finish()
            return nc
        o = O_SMALL
        Y = carve(o, [128, 17, 2, 512], BF16); o += 17 * 2 * 512 * 2
        dftC = [carve(o + i * 8704, [128, 17, 256], BF16) for i in range(2)]; o += 2 * 8704
        dftS = [carve(o + i * 8192, [128, 16, 256], BF16) for i in range(2)]; o += 2 * 8192
        tmpB = [carve(o + i * 1024, [128, 256], F32) for i in range(2)]; o += 2048
        c8 = carve(o, [128, 17, 2], BF16); o += 68
        O_WOUT = 169984
        assert o <= O_WOUT
        zfT = carve(0, [128, 4, 4096], BF16)
        O_W2 = 32768
        Win2 = carve(O_W2, [128, 8, 3072], BF16)
        WfoB = carve(O_W2 + 49152, [128, 4, D], BF16)
        WsoB = carve(O_W2 + 57344, [128, 4, D], BF16)
        assert O_W2 + 65536 <= O_SMALL
        WoutB = carve(O_WOUT, [128, 8, D], BF16)
        R_Y = [Res() for _ in range(17)]
        R_zf = Res()
        dft_r = [Res() for _ in range(2)]
        tmpB_r = [Res() for _ in range(2)]
        R_c8 = Res()
        R_w2 = Res()
        dC_v = dC.rearrange("(t p) k -> p t k", p=128)
        dS_v = dS.rearrange("(t p) k -> p t k", p=128)

        def load_dft(kb):
            s = kb % 2
            k0 = kb * 256
            dma("sp", dftC[s][:, 0:16, :], dC_v[:, :, k0:k0 + 256], "dft%d" % s, [], [dft_r[s]])
            dma("sp", dftC[s][0:1, 16, :], dCr[0:1, k0:k0 + 256], "dft%d" % s, [], [dft_r[s]])
            dma("sp", dftS[s][:, :, :], dS_v[:, :, k0:k0 + 256], "dft%d" % s, [], [dft_r[s]])

        load_dft(0)
        load_dft(1)
        dma("sp", c8[:, :, :], dC8.rearrange("p (t w) -> p t w", w=2), "c8", [], [R_c8])
        for nt in range(17):
            rows = 128 if nt < 16 else 1
            bk, br = nb()
            for g in range(4):
                mm(bk[0:rows, g * 128:(g + 1) * 128], ze[:, g, nt * 128:nt * 128 + rows], Cc, True, True,
                   [R_ze] + CR, [br], g == 3)
            cp("act", Y[0:rows, nt, 0, :], bk[0:rows, :], [br], [R_Y[nt]])
            if nt < 16:
                bk, br = nb()
                for g in range(4):
                    mm(bk[:, g * 128:(g + 1) * 128], zo[:, g, nt * 128:(nt + 1) * 128], Sc, True, True,
                       [R_zo] + CR, [br], g == 3)
                cp("dve", Y[:, nt, 1, :], bk[:, :], [br], [R_Y[nt]])
        def prefetch_w2():
            for c in range(8):
                for h in range(2):
                    dma("pool", Win2[:, c, h * 1536:(h + 1) * 1536], w_in_v[:, c, 512 + h * 1536:512 + (h + 1) * 1536], "w2",
                        [], [R_w2, R_ze, R_zo])
            dma("pool", WfoB[:, :, :], wfo.rearrange("(c p) k -> p c k", p=128), "w2", [], [R_w2, R_ze, R_zo])
            dma("pool", WsoB[:, :, :], wso.rearrange("(c p) k -> p c k", p=128), "w2", [], [R_w2, R_ze, R_zo])
            dma("pool", WoutB[:, :, :], wout.rearrange("(c p) k -> p c k", p=128), "w2", [], [R_w2])

        for kb in range(8):
            s = kb % 2
            k0 = kb * 256
            for g in range(4):
                bA, rA = nb()
                for nt in range(17):
                    rows = 128 if nt < 16 else 1
                    mm(bA[:, 0:256], Y[0:rows, nt, 0, g * 128:(g + 1) * 128], dftC[s][0:rows, nt, :], nt == 0, nt == 16,
                       [R_Y[nt], dft_r[s]], [rA], nt == 16)
                bB, rB = nb()
                for nt in range(16):
                    mm(bB[:, 0:256], Y[:, nt, 1, g * 128:(g + 1) * 128], dftS[s][:, nt, :], nt == 0, nt == 15,
                       [R_Y[nt], dft_r[s]], [rB], nt == 15)
                tb = (kb * 4 + g) % 2
                cp("act", tmpB[tb][:, :], bB[:, 0:256], [rB], [tmpB_r[tb]])
                tt("dve", zfT[:, g, k0:k0 + 256], bA[:, 0:256], tmpB[tb][:, :], ALU.subtract, [rA, tmpB_r[tb]], [R_zf])
                lo = 1 if kb == 0 else 0
                tt("dve", zfT[:, g, NTOK - k0 - lo:NTOK - k0 - 256:-1], bA[:, lo:256], tmpB[tb][:, lo:256], ALU.add,
                   [rA, tmpB_r[tb]], [R_zf])
            if kb + 2 < 8:
                load_dft(kb + 2)
            if kb == 1:
                prefetch_w2()
        for g in range(4):
            bA, rA = nb()
            for nt in range(17):
                rows = 128 if nt < 16 else 1
                mm(bA[:, 0:2], Y[0:rows, nt, 0, g * 128:(g + 1) * 128], c8[0:rows, nt, :], nt == 0, nt == 16,
                   [R_Y[nt], R_c8], [rA], nt == 16)
            cp("act", zfT[:, g, 2048:2049], bA[:, 0:1], [rA], [R_zf])
        zf_flat = zfT[:, :, :].rearrange("p a b -> p (a b)")
        dma("sp", zfd, zf_flat, "zfw", [R_zf], [])
        if dbg:
            zdbg = carve(O_SMALL, [128, 4 * 4096], F32)
            R_d = Res()
            P.barrier()
            cp("dve", zdbg[:, :], zf_flat, [R_zf], [R_d])
            dma("sp", dbg_out["d_zf"], zdbg[:, :], "dbg", [R_d], [])
        P.barrier()

        if stop == "B":
            finish()
            return nc
        zfd_v = zfd.rearrange("p (a b) -> p a b", a=4)
        o = 0
        zfs = [carve(o + i * 4096, [128, 4, 512], BF16) for i in range(2)]; o += 8192
        xn = [carve(o + i * 4096, [128, D], F32) for i in range(3)]; o += 12288
        xr = [carve(o + i * 4096, [128, D], F32) for i in range(2)]; o += 8192
        xhc = [carve(o + i * 2048, [128, D], BF16) for i in range(2)]; o += 4096
        assert o <= 32768
        o = O_SMALL
        xhT2 = [carve(o + i * 8192, [128, 8, 512], BF16) for i in range(2)]; o += 16384
        vn42 = [carve(o + i * 4096, [128, 4, 512], BF16) for i in range(2)]; o += 8192
        ug2 = [carve(o + i * 4096, [128, 4, 512], BF16) for i in range(2)]; o += 8192
        sT = carve(o, [128, 4, 512], BF16); o += 4096
        sfs = [carve(o + i * 1024, [128, 512], BF16) for i in range(4)]; o += 4096
        t12 = [carve(o + i * 2048, [128, 512], F32) for i in range(2)]; o += 4096
        vgb = carve(o, [128, 4, 128], F32); o += 2048
        sqj = carve(o, [128, 512], BF16)
        junk8 = arena[:, o // 4:o // 4 + 256].bitcast(mybir.dt.float8e4)
        o += 1024
        mergedT = carve(o, [128, 8, 512], BF16); o += 8192
        x1t_s = [carve(o + i * 4096, [128, D], F32) for i in range(2)]; o += 8192
        xh2_s = [carve(o + i * 2048, [128, D], BF16) for i in range(2)]; o += 4096
        xh2T = carve(o, [128, 8, 128], BF16); o += 2048
        ex = carve(o, [128, NE], F32); o += 64
        ex2 = carve(o, [128, NE], F32); o += 64
        assert o <= O_WOUT, o
        zfs_r = [Res() for _ in range(2)]
        xn_r = [Res() for _ in range(3)]
        xr_r = [Res() for _ in range(2)]
        xhc_r = [Res() for _ in range(2)]
        xhT2_r = [Res() for _ in range(2)]
        vn_r = [[Res() for _ in range(4)] for _ in range(2)]
        ug_r = [[Res() for _ in range(4)] for _ in range(2)]
        sT_r = [Res() for _ in range(4)]
        sfs_r = [Res() for _ in range(4)]
        t12_r = [Res() for _ in range(2)]
        R_vg = Res()
        mg_r = [Res() for _ in range(8)]
        R_x1s = [Res(), Res()]
        R_xh2s = [Res(), Res()]
        R_xh2T = Res()
        R_ex = Res()
        R_aff = Res()
        acc_r = [Res() for _ in range(32)]
        xh2d_r = [Res() for _ in range(32)]
        UO, VO, GFO, GSO = 0, 512, 1024, 2048

        def load_xn(j):
            sl = j % 3
            dma("sp", xn[sl][:, :], x[j * 128:(j + 1) * 128, :], "xt%d" % sl, [], [xn_r[sl]])

        def load_xr(j):
            sl = j % 2
            dma("sp", xr[sl][:, :], x[j * 128:(j + 1) * 128, :], "xr%d" % sl, [], [xr_r[sl]])

        def part1(st):
            s2 = st % 2
            n0 = st * 512

            rsC = {}

            def hA(i):
                def f():
                    j = st * 4 + i
                    if i == 0:
                        dma("sp", zfs[s2][:, :, :], zfd_v[:, :, n0:n0 + 512], "zfs%d" % s2, [], [zfs_r[s2]])
                    rsC[i] = rms_head(xn[j % 3][:, :], xn_r[j % 3], junk8, 16 + (j % 8) * 2, saturate=False)
                return f

            def tA(i):
                def f():
                    j = st * 4 + i
                    rms_tail(xn[j % 3][:, :], xn_r[j % 3], xhc[i % 2][:, :], xhc_r[i % 2], g1bc[:, :], 16 + (j % 8) * 2, rsC.pop(i))
                    if j + 3 < 32:
                        load_xn(j + 3)
                return f

            def cB(i):
                def f():
                    transposes(xhc[i % 2], xhc_r[i % 2], xhT2[s2], xhT2_r[s2], i)
                return f

            def cVa(i):
                def f():
                    bk, br = nb()
                    for c in range(8):
                        mm(bk[:, :], xhT2[s2][:, c, i * 128:(i + 1) * 128], Win2[:, c, VO:VO + 512], c == 0, c == 7,
                           [xhT2_r[s2], R_w2], [br], c == 7)
                    vg2 = vgb[:, :, :].rearrange("p a b -> p (a b)")
                    act(vg2, bk[:, :], AF.Gelu_apprx_tanh, [br], [R_vg])
                    s1 = small[:, 40:44]
                    s2c = small[:, 44:48]
                    red("dve", s1, vgb[:, :, :], ALU.add, [R_vg], [R_vg])
                    ts1("dve", s1, s1, 1.0 / 128, ALU.mult, [R_vg], [R_vg])
                    tt("dve", vgb[:, :, :], vgb[:, :, :], s1.unsqueeze(2).to_broadcast([128, 4, 128]), ALU.subtract, [R_vg], [R_vg])
                    for g in range(4):
                        act(sqj[:, g * 128:(g + 1) * 128], vgb[:, g, :], AF.Square, [R_vg], [R_vg], scale=float(128.0 ** -0.5),
                            accum_out=s2c[:, g:g + 1])
                    act(s2c, s2c, AF.Identity, [R_vg], [R_vg], scale=1.0, bias=EPS)
                    P.op("pool", lambda e: e.tensor_tensor(out=s2c, in0=s2c, in1=mhalf[:, 0:4], op=ALU.pow), [R_vg] + CR, [R_vg])
                return f

            def cVb(i):
                def f():
                    vg2 = vgb[:, :, :].rearrange("p a b -> p (a b)")
                    s2c = small[:, 44:48]
                    for g in range(4):
                        stt("dve", vgb[:, g, :], vgb[:, g, :], s2c[:, g:g + 1], lngbc[:, g * 128:(g + 1) * 128], ALU.mult, ALU.mult,
                            [R_vg] + CR, [R_vg])
                    tt("dve", vn42[s2][:, i, :], vg2, lnbbc[:, :], ALU.add, [R_vg] + CR, [vn_r[s2][i]])
                return f

            def cU(cb):
                def f():
                    bk, br = nb()
                    for c in range(8):
                        mm(bk[:, :], Win2[:, c, UO + cb * 128:UO + (cb + 1) * 128], xhT2[s2][:, c, :], c == 0, c == 7,
                           [xhT2_r[s2], R_w2], [br], c == 7)
                    act(ug2[s2][:, cb, :], bk[:, :], AF.Gelu_apprx_tanh, [br], [ug_r[s2][cb]])
                return f

            def seq(*fs):
                def f():
                    for g_ in fs:
                        g_()
                return f

            nop = lambda: None
            AB = [hA(0), seq(hA(1), tA(0)), seq(hA(2), tA(1), cB(0)), seq(hA(3), tA(2), cB(1)), seq(tA(3), cB(2)), cB(3), nop, nop]
            return dict(AB=AB, Va=[cVa(i) for i in range(4)], Vb=[cVb(i) for i in range(4)], U=[cU(cb) for cb in range(4)])

        def part2(st):
            s2 = st % 2
            n0 = st * 512

            def cS(g):
                def f():
                    bk, br = nb()
                    for i in range(4):
                        mm(bk[:, i * 128:(i + 1) * 128], vn42[s2][:, i, g * 128:(g + 1) * 128], wsTb[:, g * 128:(g + 1) * 128],
                           True, False, [vn_r[s2][i]] + CR, [br], False)
                        mm(bk[:, i * 128:(i + 1) * 128], onesb[0:1, :], bsb[0:1, g * 128:(g + 1) * 128],
                           False, True, CR, [br], i == 3)
                    tt("dve", sT[:, g, :], bk[:, :], ug2[s2][:, g, :], ALU.mult, [br, ug_r[s2][g]], [sT_r[g]])
                return f

            def cG(db):
                def f():
                    bgf, rgf = nb()
                    for c in range(8):
                        mm(bgf[:, :], Win2[:, c, GFO + db * 128:GFO + (db + 1) * 128], xhT2[s2][:, c, :], c == 0, c == 7,
                           [xhT2_r[s2], R_w2], [rgf], c == 7)
                    bgs, rgs = nb()
                    for c in range(8):
                        mm(bgs[:, :], Win2[:, c, GSO + db * 128:GSO + (db + 1) * 128], xhT2[s2][:, c, :], c == 0, c == 7,
                           [xhT2_r[s2], R_w2], [rgs], c == 7)
                    byf, ryf = nb()
                    for g in range(4):
                        mm(byf[:, :], WfoB[:, g, db * 128:(db + 1) * 128], zfs[s2][:, g, :], g == 0, g == 3,
                           [zfs_r[s2], R_w2], [ryf], g == 3)
                    bys, rys = nb()
                    for g in range(4):
                        mm(bys[:, :], WsoB[:, g, db * 128:(db + 1) * 128], sT[:, g, :], g == 0, g == 3,
                           [sT_r[g], R_w2], [rys], g == 3)
                    q = (db % 2) * 2
                    act(sfs[q][:, :], bgf[:, :], AF.Sigmoid, [rgf], [sfs_r[q]])
                    act(sfs[q + 1][:, :], bgs[:, :], AF.Sigmoid, [rgs], [sfs_r[q + 1]])
                    tt("dve", t12[0][:, :], byf[:, :], sfs[q][:, :], ALU.mult, [ryf, sfs_r[q]], [t12_r[0]])
                    tt("dve", t12[1][:, :], bys[:, :], sfs[q + 1][:, :], ALU.mult, [rys, sfs_r[q + 1]], [t12_r[1]])
                    tt("pool", mergedT[:, db, :], t12[0][:, :], t12[1][:, :], ALU.add, [t12_r[0], t12_r[1]], [mg_r[db]])
                return f

            rsX = {}

            def cX1(i):
                def f():
                    j = st * 4 + i
                    sl = j % 2
                    x1t, xh2, R_x1, R_xh2 = x1t_s[sl], xh2_s[sl], R_x1s[sl], R_xh2s[sl]
                    for hf in range(2):
                        bk, br = nb()
                        for c in range(8):
                            mm(bk[:, :], mergedT[:, c, i * 128:(i + 1) * 128], WoutB[:, c, hf * 512:(hf + 1) * 512], c == 0, c == 7,
                               [mg_r[c], R_w2], [br], c == 7)
                        tt("dve", x1t[:, hf * 512:(hf + 1) * 512], bk[:, :], xr[sl][:, hf * 512:(hf + 1) * 512], ALU.add,
                           [br, xr_r[sl]], [R_x1])
                    if j + 2 < 32:
                        load_xr(j + 2)
                    dma("sp", acc[j * 128:(j + 1) * 128, :], x1t[:, :], "accw", [R_x1], [acc_r[j]])
                    rsX[i] = rms_head(x1t[:, :], R_x1, junk8, 32 + (j % 4) * 2, saturate=False)
                return f

            def cXT(i):
                def f():
                    j = st * 4 + i
                    sl = j % 2
                    x1t, xh2, R_x1, R_xh2 = x1t_s[sl], xh2_s[sl], R_x1s[sl], R_xh2s[sl]
                    rms_tail(x1t[:, :], R_x1, xh2[:, :], R_xh2, g2bc[:, :], 32 + (j % 4) * 2, rsX.pop(i))
                    dma("sp", xh2d[j * 128:(j + 1) * 128, :], xh2[:, :], "xh2w", [R_xh2], [xh2d_r[j]])
                return f

            def cX2a(i):
                def f():
                    j = st * 4 + i
                    sl = j % 2
                    x1t, xh2, R_x1, R_xh2 = x1t_s[sl], xh2_s[sl], R_x1s[sl], R_xh2s[sl]
                    transposes(xh2, R_xh2, xh2T, R_xh2T, 0)
                return f

            def cX2l(i):
                def f():
                    bk, br = nb()
                    for c in range(8):
                        mm(bk[:, 0:NE], xh2T[:, c, :], wrb[:, c, :], c == 0, c == 7, [R_xh2T] + CR, [br], c == 7)
                    mx = small[:, 48:49]
                    red("dve", mx, bk[:, 0:NE], ALU.max, [br], [R_ex])
                    ts1("dve", mx, mx, -0.5, ALU.mult, [R_ex], [R_ex])
                    act(ex[:, :], bk[:, 0:NE], AF.Tanh, [br, R_ex], [R_ex], bias=mx, scale=0.5)
                return f

            def cX2b(i):
                def f():
                    j = st * 4 + i
                    sm = small[:, 49:50]
                    ts("dve", ex2[:, :], ex[:, :], -1.0, 1.0, ALU.mult, ALU.add, [R_ex], [R_ex])
                    recip(ex2[:, :], ex2[:, :], [R_ex], [R_ex])
                    stt("dve", ex[:, :], ex[:, :], 1.0, ex2[:, :], ALU.add, ALU.mult, [R_ex], [R_ex])
                    red("dve", sm, ex[:, :], ALU.add, [R_ex], [R_ex])
                    recip(sm, sm, [R_ex], [R_ex])
                    ts1("dve", aff[:, j, :], ex[:, :], sm, ALU.mult, [R_ex], [R_aff])
                return f

            return dict(S=[cS(g) for g in range(4)], G=[cG(db) for db in range(8)], X1=[cX1(i) for i in range(4)],
                        X2a=[cX2a(i) for i in range(4)], X2b=[cX2b(i) for i in range(4)], XT=[cXT(i) for i in range(4)],
                        X2l=[cX2l(i) for i in range(4)])

        for j in range(3):
            load_xn(j)
        load_xr(0)
        load_xr(1)
        def run(lst):
            for f in lst:
                f()

        p1 = part1(0)
        p2 = part2(0)
        run(p1["AB"])
        for i in range(4):
            p1["Va"][i]()
            p1["U"][i]()
            p1["Vb"][i]()
        for g in range(4):
            p2["S"][g]()
        nopl = [lambda: None] * 4
        for sti in range(8):
            nxt = sti + 1 < 8
            p1n = part1(sti + 1) if nxt else None
            p2n = part2(sti + 1) if nxt else None
            for k in range(8):
                p2["G"][k]()
                if nxt:
                    p1n["AB"][k]()
            X1, Xa, Xb, T, Xl = p2["X1"], p2["X2a"], p2["X2b"], p2["XT"], p2["X2l"]
            Va = p1n["Va"] if nxt else nopl
            Vb = p1n["Vb"] if nxt else nopl
            for f in (X1[0], Va[0], T[0], X1[1], Vb[0], Xa[0], Va[1], T[1], X1[2], Xl[0], Vb[1], Xb[0], Xa[1], Va[2], T[2], X1[3],
                      Xl[1], Vb[2], Xb[1], Xa[2], Va[3], T[3], Xl[2], Vb[3], Xb[2], Xa[3], Xl[3], Xb[3]):
                f()
            if nxt:
                for g in range(4):
                    p1n["U"][g]()
                    p2n["S"][g]()
            p2 = p2n
        P.barrier()
        if stop == "C":
            finish()
            return nc
        o = 0
        affP = carve(o, [128, 512], F32); o += 2048
        junk = carve(o, [128, 512], BF16); o += 1024
        maskb = carve(o, [128, 512], BF16); o += 1024
        posS = carve(o, [128, 512], F32); o += 2048
        aS = carve(o, [128, 512], F32); o += 2048
        bS = carve(o, [128, 32, NE], F32); o += 2048
        m2 = carve(o, [128, 512], F32); o += 2048
        eqm = carve(o, [128, 512, 4], F32); o += 8192
        vals4 = carve(o, [128, 512, 4], F32); o += 8192
        OT = 149504
        ahb = carve(OT, [128, 512], BF16)
        A4 = carve(OT + 1024, [128, 512, 16], BF16)
        Bm = [carve(OT + 17408 + i * 8192, [128, 32, 128], BF16) for i in range(2)]
        resS = carve(OT + 33792, [128, 4, 4], F32)
        idxf = carve(OT + 33856, [128, 4], F32)
        assert OT + 33872 <= ARENA_W * 4
        thrS = carve(o, [128, NE], F32); o += 64
        diag = carve(o, [16, NE], F32); o += 64
        bis = carve(o, [128, 8], F32); o += 32
        PH_D_END = o
        R_affT = Res()
        R_b = Res()
        aff2 = aff[:, :, :].rearrange("p a b -> p (a b)")
        bk, br = nb()
        for q in range(4):
            mm(bk[:, q * 128:(q + 1) * 128], aff2[:, q * 128:(q + 1) * 128], identF, True, True, [R_aff] + CR, [br], q == 3)
        cp("act", affP[:, :], bk[:, :], [br], [R_affT])
        lo_, hi_, mid_, cnt_, flg_, d1_ = (bis[:, i:i + 1] for i in range(6))
        P.op("dve", lambda e: e.memset(bis[:, :], 0.0), [], [R_b])
        for it in range(24):
            hk = 2.0 ** -(it + 1)
            ts1("dve", mid_, lo_, hk, ALU.add, [R_b], [R_b])
            ts("dve", junk[:, :], affP[:, :], mid_, 0.0, ALU.is_ge, ALU.add, [R_b, R_affT], [R_b], accum_out=cnt_)
            bk, br = nb()
            mm(bk[:, 0:2], Bsum, bis[:, 3:5], True, True, [R_b] + CR, [br], True)
            ts("dve", flg_, bk[:, 0:1], CAP - 0.5, hk, ALU.is_gt, ALU.mult, [br, R_b], [R_b])
            tt("dve", lo_, lo_, flg_, ALU.add, [R_b], [R_b])
        ts1("dve", diag[:, :], identF[0:16, 0:16], bis[0:16, 0:1], ALU.mult, [R_b] + CR, [R_b])
        bk, br = nb()
        mm(bk[:, 0:NE], onesF[0:16, :], diag[:, :], True, True, [R_b] + CR, [br], True)
        R_m = Res()
        cp("dve", thrS[:, :], bk[:, 0:NE], [br], [R_m])
        if dbg:
            dma("sp", dbg_out["d_thr"], bis[0:16, 0:4], "dbg", [R_b], [])
            dma("sp", dbg_out["d_aff"], aff2, "dbg", [R_aff], [])
        mask3 = maskb[:, :].rearrange("p (a b) -> p a b", a=32)
        tt("dve", mask3, aff[:, :, :], thrS[:, :].unsqueeze(1).to_broadcast([128, 32, NE]), ALU.is_ge, [R_aff, R_m], [R_m])
        bk, br = nb()
        bk3 = bk[:, :].rearrange("p (a b) -> p a b", a=32)
        mm(bk[:, :], tri, maskb[:, :], True, False, [R_m] + CR, [br], False)
        for jp in range(31):
            mm(bk3[:, jp + 1:32, :], onesb, mask3[:, jp, :].unsqueeze(1).to_broadcast([128, 31 - jp, NE]), False, jp == 30,
               [R_m] + CR, [br], jp == 30)
        cp("dve", posS[:, :], bk[:, :], [br], [R_m])
        ts1("dve", aS[:, :], posS[:, :], 128.0, ALU.is_ge, [R_m], [R_m])
        stt("dve", aS[:, :], posS[:, :], 256.0, aS[:, :], ALU.is_ge, ALU.add, [R_m], [R_m])
        stt("dve", aS[:, :], posS[:, :], 384.0, aS[:, :], ALU.is_ge, ALU.add, [R_m], [R_m])
        bS2 = bS[:, :, :].rearrange("p a b -> p (a b)")
        stt("dve", bS2, aS[:, :], -128.0, posS[:, :], ALU.mult, ALU.add, [R_m], [R_m])
        ts1("dve", m2[:, :], posS[:, :], float(CAP), ALU.is_lt, [R_m], [R_m])
        tt("dve", m2[:, :], m2[:, :], maskb[:, :], ALU.mult, [R_m], [R_m])
        for ca in range(4):
            stt("dve", eqm[:, :, ca], aS[:, :], float(ca), m2[:, :], ALU.is_equal, ALU.mult, [R_m], [R_m])
        v44 = vals4[:, :, :].rearrange("p (a b) c -> p a b c", a=32)
        cp("dve", v44[:, :, :, 0], thi.unsqueeze(2).to_broadcast([128, 32, NE]), [R_m] + CR, [R_m])
        cp("dve", v44[:, :, :, 1], tlo.unsqueeze(2).to_broadcast([128, 32, NE]), [R_m] + CR, [R_m])
        cp("dve", ahb[:, :], aff2, [R_aff, R_m], [R_m])
        cp("dve", vals4[:, :, 2], ahb[:, :], [R_m], [R_m])
        tt("dve", vals4[:, :, 3], aff2, ahb[:, :], ALU.subtract, [R_aff, R_m], [R_m])
        A44 = A4[:, :, :].rearrange("p a (b c) -> p a b c", b=4)
        for ca in range(4):
            tt("dve", A44[:, :, ca, :], vals4[:, :, :], eqm[:, :, ca].unsqueeze(2).to_broadcast([128, 512, 4]), ALU.mult,
               [R_m], [R_m])
        A4e = A4[:, :, :].rearrange("p (a b) c -> p a b c", a=32)
        Bm_r = [Res() for _ in range(2)]
        R_res = Res()
        R_gi = Res()
        R_gie = [Res() for _ in range(NE)]
        bSb = ahb[:, :].rearrange("p (a b) -> p a b", a=32)
        cp("dve", ahb[:, :], bS2, [R_m], [R_m])

        def buildB(e_):
            s = e_ % 2
            tt("dve", Bm[s][:, :, :], iotaB.unsqueeze(1).to_broadcast([128, 32, 128]),
               bSb[:, :, e_].unsqueeze(2).to_broadcast([128, 32, 128]), ALU.is_equal, [R_m] + CR, [Bm_r[s]])

        o = 0
        WdB = [carve(o + i * 32768, [128, 16, D], BF16) for i in range(2)]; o += 65536
        hT = carve(o, [128, 16, 512], BF16); o += 16384
        xe = [carve(o + i * 8192, [128, 4, D], BF16) for i in range(2)]; o += 16384
        xeT = carve(o, [128, 8, 512], BF16); o += 8192
        ye = [carve(o + i * 4096, [128, D], F32) for i in range(2)]; o += 8192
        sa = [carve(o + i * 1024, [128, 512], BF16) for i in range(2)]; o += 2048
        NB = 4
        WgB = [carve(o + i * 4096, [128, 8, 256], BF16) for i in range(NB)]; o += NB * 4096
        WuB = [carve(o + i * 4096, [128, 8, 256], BF16) for i in range(NB)]; o += NB * 4096
        assert o <= OT
        WdB_r = [[Res() for _ in range(8)] for _ in range(2)]
        hT_r = [Res() for _ in range(16)]
        xe_r = [Res() for _ in range(2)]
        xeT_r = [Res() for _ in range(8)]
        ye_r = [Res() for _ in range(2)]
        sa_r = [Res() for _ in range(2)]
        WgB_r = [Res() for _ in range(NB)]
        WuB_r = [Res() for _ in range(NB)]
        NST = NE * 8
        PD = 3

        def w_load(s, parts="gud"):
            e_, st_ = divmod(s, 8)
            b = s % NB
            f0 = st_ * 256
            if "d" in parts:
                dma("pool", WdB[e_ % 2][:, st_ * 2:st_ * 2 + 2, :], wd[e_][f0:f0 + 256, :].rearrange("(a p) d -> p a d", p=128),
                    "wd%d" % (e_ % 2), [], [WdB_r[e_ % 2][st_]])
            if "g" not in parts:
                return
            dma("pool", WgB[b][:, :, :].rearrange("p c f -> p (c f)"), wg[s * 128:(s + 1) * 128, :], "wg%d" % b, [], [WgB_r[b]])
            dma("pool", WuB[b][:, :, :].rearrange("p c f -> p (c f)"), wu[s * 128:(s + 1) * 128, :], "wu%d" % b, [], [WuB_r[b]])

        def gather(e_):
            s = e_ % 2
            for ca in range(4):
                P.dma("pool", lambda e, s=s, ca=ca, e_=e_: e.indirect_dma_start(
                    out=xe[s][:, ca, :], out_offset=None, in_=xh2d,
                    in_offset=bass.IndirectOffsetOnAxis(ap=idxs[:, e_, ca:ca + 1], axis=0)),
                    "ga%d" % s, [R_gie[e_]] + xh2d_r, [xe_r[s]])

        assert PH_D_END <= 65536
        def idxM(e_):
            s = e_ % 2
            bk, br = nb()
            for j in range(32):
                mm(bk[:, 0:16], Bm[s][:, j, :], A4e[:, j, e_, :], j == 0, j == 31, [Bm_r[s], R_m], [br], j == 31)
            cp("act", resS[:, :, :].rearrange("p a b -> p (a b)"), bk[:, 0:16], [br], [R_res])
            stt("dve", idxf[:, :], resS[:, :, 0], 64.0, resS[:, :, 1], ALU.mult, ALU.add, [R_res], [R_res])
            cp("dve", idxs[:, e_, :], idxf[:, :], [R_res], [R_gie[e_]])
            tt("dve", gates[:, e_, :], resS[:, :, 2], resS[:, :, 3], ALU.add, [R_res], [R_gie[e_]])

        for s_ in range(PD):
            w_load(s_, "gu")
        buildB(0)
        idxM(0)
        gather(0)
        P._deps("pool", [R_m, R_b, R_affT], [R_m, R_b, R_affT])

        if stop == "D":
            finish()
            return nc
        sc_prev = [None]
        sc_cur = [None]
        P.dsem("sc")
        for s in range(PD):
            w_load(s, "d")
        for e_ in range(NE):
            xs = e_ % 2
            for c in range(8):
                bk, br = nb()
                for ca in range(4):
                    mm(bk[:, ca * 128:(ca + 1) * 128], xe[xs][:, ca, c * 128:(c + 1) * 128], ident, True, True,
                       [xe_r[xs]] + CR, [br], ca == 3)
                cp("act" if c % 2 == 0 else "dve", xeT[:, c, :], bk[:, :], [br], [xeT_r[c]])
            if e_ + 1 < NE:
                buildB(e_ + 1)
            for st_ in range(8):
                s = e_ * 8 + st_
                b = s % NB
                if st_ == 2 and e_ + 1 < NE:
                    idxM(e_ + 1)
                if s + PD < NST:
                    w_load(s + PD)
                if st_ == 5 and e_ + 1 < NE:
                    gather(e_ + 1)
                for h in range(2):
                    fb = st_ * 2 + h
                    bA, rA = nb()
                    for c in range(8):
                        mm(bA[:, :], WgB[b][:, c, h * 128:(h + 1) * 128], xeT[:, c, :], c == 0, c == 7,
                           [WgB_r[b], xeT_r[c]], [rA], c == 7)
                    bG, rG = nb()
                    for c in range(8):
                        mm(bG[:, :], WuB[b][:, c, h * 128:(h + 1) * 128], xeT[:, c, :], c == 0, c == 7,
                           [WuB_r[b], xeT_r[c]], [rG], c == 7)
                    q = fb % 2
                    act(sa[q][:, :], bA[:, :], AF.Silu, [rA], [sa_r[q]])
                    tt("dve", hT[:, fb, :], bG[:, :], sa[q][:, :], ALU.mult, [rG, sa_r[q]], [hT_r[fb]])
            for ca in range(4):
                ys = ca % 2
                for hf in range(2):
                    bk, br = nb()
                    for fb in range(16):
                        mm(bk[:, :], hT[:, fb, ca * 128:(ca + 1) * 128], WdB[e_ % 2][:, fb, hf * 512:(hf + 1) * 512], fb == 0, fb == 15,
                           [hT_r[fb]] + WdB_r[e_ % 2], [br], fb == 15)
                    ts1("dve", ye[ys][:, hf * 512:(hf + 1) * 512], bk[:, :], gates[:, e_, ca:ca + 1], ALU.mult,
                        [br, R_gie[e_]], [ye_r[ys]])
                if sc_prev[0] is not None:
                    P._wait("pool", "sc", sc_prev[0])
                tok = P.dma("pool", lambda e, ys=ys, ca=ca, e_=e_: e.indirect_dma_start(
                    out=acc, out_offset=bass.IndirectOffsetOnAxis(ap=idxs[:, e_, ca:ca + 1], axis=0),
                    in_=ye[ys][:, :], in_offset=None, compute_op=ALU.add),
                    "sc", [R_gie[e_], ye_r[ys]], [])
                sc_cur[0] = tok[1]
            sc_prev[0] = sc_cur[0]
            for r_ in acc_r:
                r_.w = ("sc", sc_cur[0])
                r_.r = []
        P.barrier()

        if dbg:
            R_d2 = Res()
            dbgi = carve(0, [128, 64], F32)
            cp("dve", dbgi[:, :], idxs[:, :, :].rearrange("p a b -> p (a b)"), R_gie, [R_d2])
            dma("sp", dbg_out["d_idx"], dbgi[:, :], "dbg", [R_d2], [])
            dma("sp", dbg_out["d_gate"], gates[:, :, :].rearrange("p a b -> p (a b)"), "dbg", R_gie, [])
        if dbg:
            P.barrier()
        if stop == "E":
            finish()
            return nc
        o = 0
        fgbc = carve(o, [128, D], F32); o += 4096
        NXF, NYF, PF = 6, 6, 5
        xf = [carve(o + i * 4096, [128, D], F32) for i in range(NXF)]; o += NXF * 4096
        yf = [carve(o + i * 4096, [128, D], F32) for i in range(NYF)]; o += NYF * 4096
        jf = carve(o, [128, D], BF16); o += 2048
        xf_r = [Res() for _ in range(NXF)]
        yf_r = [Res() for _ in range(NYF)]
        R_fg = Res()
        dma("sp", fgbc[:, :], fg.partition_broadcast(128), "fgc", [], [R_fg])

        def load_f(j):
            sl = j % NXF
            dma("sp", xf[sl][:, :], acc[j * 128:(j + 1) * 128, :], "xf%d" % sl, [acc_r[j]], [xf_r[sl]])

        for j in range(PF):
            load_f(j)
        last = []
        for j in range(32 + 2):
            if j < 32:
                sl = j % NXF
                ys = j % NYF
                col = (j % 8) * 2
                ss = small[:, col:col + 1]
                rs = small[:, col + 1:col + 2]
                R_s = Res()
                act(jf[:, :], xf[sl][:, :], AF.Square, [xf_r[sl]], [R_s], scale=1.0 / 32.0, accum_out=ss)
                act(ss, ss, AF.Identity, [R_s], [R_s], scale=1.0, bias=EPS)
                P.op("pool", lambda e, rs=rs, ss=ss: e.tensor_tensor(out=rs, in0=ss, in1=mhalf[:, 0:1], op=ALU.pow), [R_s] + CR, [R_s])
                stt("dve", yf[ys][:, :], xf[sl][:, :], rs, fgbc[:, :], ALU.mult, ALU.mult, [xf_r[sl], R_s, R_fg], [yf_r[ys]])
                if j + PF < 32:
                    load_f(j + PF)
            k = j - 2
            if 0 <= k < 32:
                ks = k % NYF
                t = dma("sp", out[k * 128:(k + 1) * 128, :], yf[ks][:, :], "ost%d" % (k % 2), [yf_r[ks]], [Res()])
                last.append(t)
        for t in last[-2:]:
            P._wait("sp", t[0], t[1])
        P.barrier()

        finish()
    return nc


_CACHE = {}


def _consts():
    if "c" in _CACHE:
        return _CACHE["c"]
    bf = ml_dtypes.bfloat16
    p = np.arange(128)
    ident = np.eye(128, dtype=np.float32)
    tri = (p[:, None] < p[None, :]).astype(np.float32)
    ones = np.ones((128, 128), np.float32)
    ang = 2.0 * np.pi * ((p[:, None] * p[None, :]) % 128) / 128.0
    Cc = np.cos(ang) / np.sqrt(128.0)
    Sc = np.sin(ang) / np.sqrt(128.0)
    iota = np.broadcast_to(p[None, :].astype(np.float32), (128, 128))
    cbf = np.concatenate([ident, tri, ones, Cc, Sc, iota], axis=1).astype(bf)
    t = 128 * np.arange(32)[None, :] + p[:, None]
    bsum = ((p[:, None] % 16) == (p[None, :] % 16)).astype(np.float32)
    cf = np.concatenate([ident, iota, (t // 64).astype(np.float32), (t % 64).astype(np.float32), bsum], axis=1).astype(np.float32)
    n = np.arange(2049, dtype=np.int64)
    ph = (n[:, None] * n[None, :]) % 4096
    a2 = 2.0 * np.pi * ph.astype(np.float64) / 4096.0
    Cs = np.cos(a2) / 64.0
    Ss = np.sin(a2) / 64.0
    dC = Cs[0:2048, 0:2048].astype(bf)
    dCr = Cs[2048:2049, 0:2048].astype(bf)
    col = np.zeros((17 * 128,), np.float64)
    col[0:2049] = Cs[:, 2048]
    c8 = np.zeros((128, 17, 2), np.float64)
    c8[:, :, 0] = col.reshape(17, 128).T
    dC8 = c8.reshape(128, 34).astype(bf)
    dS = Ss[0:2048, 0:2048].astype(bf)
    c = dict(cbf=np.ascontiguousarray(cbf), cf=np.ascontiguousarray(cf), dC=np.ascontiguousarray(dC),
             dCr=np.ascontiguousarray(dCr), dC8=np.ascontiguousarray(dC8), dS=np.ascontiguousarray(dS))
    _CACHE["c"] = c
    return c


def _in_maps(inputs, cores):
    f = lambda a: np.ascontiguousarray(np.asarray(a, dtype=np.float32))
    x = f(inputs["x"])

    def relay(w):
        w = np.asarray(w, dtype=np.float32).reshape(NE, 8, 128, 8, 256)
        return np.ascontiguousarray(np.transpose(w, (0, 3, 2, 1, 4))).reshape(NE * 8 * 128, 2048)

    shared = dict(
        w_in=f(inputs["w_in"][0]), w_fo=f(inputs["w_fourier_out"][0]), w_so=f(inputs["w_sgu_out"][0]),
        w_out=f(inputs["w_out"][0]), w_r=f(inputs["w_router"][0]), wg=relay(inputs["w_gate_e"][0]),
        wu=relay(inputs["w_up_e"][0]), wd=f(inputs["w_down_e"][0]), g1=f(inputs["norm1_g"][0]),
        g2=f(inputs["norm2_g"][0]), fg=f(inputs["final_g"]), lng=f(inputs["sgu_ln_g"][0]), lnb=f(inputs["sgu_ln_b"][0]),
        wsT=f(np.transpose(np.asarray(inputs["w_spatial"][0]), (2, 0, 1)).reshape(128, 512)),
        bs=f(np.asarray(inputs["b_spatial"][0]).reshape(1, 512)),
    )
    shared.update(_consts())
    maps = []
    for b in cores:
        m = dict(shared)
        m["x"] = np.ascontiguousarray(x[b])
        maps.append(m)
    return maps


def kernel(**inputs):
    if "nc" not in _CACHE:
        _CACHE["nc"] = build(False)
    nc = _CACHE["nc"]
    maps = _in_maps(inputs, list(range(8)))
    res = run_bass_kernel_spmd(nc, maps, core_ids=list(range(8)))
    return np.stack([np.asarray(r["out"], dtype=np.float32) for r in res.results], axis=0)
```
